# Optimizing a Trainium2 kernel written in Bass

```python
import math
import jax
import jax.numpy as jnp
from jax import lax
import numpy as np

D_MODEL = 2048
BATCH = 4
SEQ = 4096
DEPTH = 2

HEAD_DIM = 128
FNET_GROUPS = 4
FNET_GROUP_DIM = 128
FNET_WIDTH = FNET_GROUPS * FNET_GROUP_DIM
DIL_PATTERNS = ((128, 1), (512, 4), (2048, 16))
DIL_GROUPS = len(DIL_PATTERNS)
DIL_HEADS = 4
DIL_RADII = tuple((w // 2) // d for w, d in DIL_PATTERNS)
DIL_BLOCK = max(DIL_RADII)
DIL_QKV_WIDTH = DIL_GROUPS * DIL_HEADS * HEAD_DIM
DIL_OUT_WIDTH = DIL_HEADS * HEAD_DIM
DIFF_HEADS = 4
DIFF_QK_WIDTH = DIFF_HEADS * 2 * HEAD_DIM
DIFF_V_DIM = 2 * HEAD_DIM
DIFF_V_WIDTH = DIFF_HEADS * DIFF_V_DIM
DIFF_Q_BLOCK = 128
N_BRANCHES = 3
COL_FNET = 0
COL_DIL = COL_FNET + FNET_WIDTH
COL_DIFF = COL_DIL + 3 * DIL_QKV_WIDTH
COL_GATE = COL_DIFF + 2 * DIFF_QK_WIDTH + DIFF_V_WIDTH
IN_WIDTH = COL_GATE + N_BRANCHES * D_MODEL
REL_BUCKETS = 32
REL_MAX_DISTANCE = 2048
REL_HEADS = DIL_GROUPS * DIL_HEADS + DIFF_HEADS
PEER_HEADS = 8
PEER_NKEYS = 128
PEER_EXPERTS = PEER_NKEYS * PEER_NKEYS
PEER_TOPK = 16
PEER_QDIM = 256
PEER_CHUNK = 64
RMS_EPS = 1e-6
NEG_INF = -1e30

kernel_name = "hybrid_fnet_dilated_diffattn_peer_encoder"


def rms_norm(x, g):
    xf = x.astype(jnp.float32)
    xf = xf * lax.rsqrt(jnp.mean(xf * xf, axis=-1, keepdims=True) + RMS_EPS)
    return (xf * g.astype(jnp.float32)).astype(x.dtype)


def rel_bucket(rel):
    half = REL_BUCKETS // 2
    max_exact = half // 2
    n = jnp.abs(rel)
    big = max_exact + (jnp.log(jnp.maximum(n, 1).astype(jnp.float32) / max_exact)
                       / math.log(REL_MAX_DISTANCE / max_exact) * (half - max_exact)).astype(jnp.int32)
    big = jnp.minimum(big, half - 1)
    return jnp.where(rel > 0, half, 0) + jnp.where(n < max_exact, n, big)


def fourier_mixer(a):
    b, s, _ = a.shape
    z = a.reshape(b, s, FNET_GROUPS, FNET_GROUP_DIM).astype(jnp.float32)
    y = jnp.fft.fft2(z, axes=(1, 3), norm='ortho').real
    return y.reshape(b, s, FNET_WIDTH).astype(a.dtype)


def dilated_group(q, k, v, dilation, radius, bias_tab):
    b, s, h, dh = q.shape
    sub_len = s // dilation
    n_blk = -(-sub_len // DIL_BLOCK)
    pad_len = n_blk * DIL_BLOCK

    def to_sub(t):
        t = t.reshape(b, sub_len, dilation, h, dh).transpose(0, 2, 1, 3, 4)
        return jnp.pad(t, ((0, 0), (0, 0), (0, pad_len - sub_len), (0, 0), (0, 0)))

    def neighbours(t):
        t = jnp.pad(to_sub(t), ((0, 0), (0, 0), (DIL_BLOCK, DIL_BLOCK), (0, 0), (0, 0)))
        t = t.reshape(b, dilation, n_blk + 2, DIL_BLOCK, h, dh)
        return jnp.concatenate([t[:, :, :-2], t[:, :, 1:-1], t[:, :, 2:]], axis=3)

    qs = to_sub(q).reshape(b, dilation, n_blk, DIL_BLOCK, h, dh)
    ks = neighbours(k)
    vs = neighbours(v)
    a_idx = jnp.arange(DIL_BLOCK, dtype=jnp.int32)
    c_idx = jnp.arange(3 * DIL_BLOCK, dtype=jnp.int32)
    offset = c_idx[None, :] - DIL_BLOCK - a_idx[:, None]
    key_m = (jnp.arange(n_blk, dtype=jnp.int32)[:, None] - 1) * DIL_BLOCK + c_idx[None, :]
    valid = (jnp.abs(offset) <= radius)[None] & ((key_m >= 0) & (key_m < sub_len))[:, None, :]
    bias = bias_tab[rel_bucket(offset * dilation)].astype(jnp.float32).transpose(2, 0, 1)
    logits = jnp.einsum('brnahe,brnche->brnhac', qs, ks,
                        preferred_element_type=jnp.float32) * (dh ** -0.5) + bias
    logits = jnp.where(valid[:, None], logits, NEG_INF)
    mx = jnp.max(logits, axis=-1, keepdims=True)
    p = jnp.exp(logits - mx)
    den = jnp.sum(p, axis=-1, keepdims=True)
    o = jnp.einsum('brnhac,brnche->brnahe', p / den, vs.astype(jnp.float32))
    lse = jnp.swapaxes((mx + jnp.log(den))[..., 0], 3, 4)

    def from_sub(t):
        t = t.reshape(b, dilation, pad_len, *t.shape[4:])[:, :, :sub_len]
        return jnp.swapaxes(t, 1, 2).reshape(b, s, *t.shape[3:])

    return from_sub(o), from_sub(lse)


def dilated_mixer(q, k, v, bias_tab):
    outs, lses = [], []
    for g, (window, dilation) in enumerate(DIL_PATTERNS):
        o, lse = dilated_group(q[:, :, g], k[:, :, g], v[:, :, g], dilation, DIL_RADII[g],
                               bias_tab[:, g * DIL_HEADS:(g + 1) * DIL_HEADS])
        outs.append(o)
        lses.append(lse)
    w = jax.nn.softmax(jnp.stack(lses, axis=2), axis=2)
    return jnp.sum(jnp.stack(outs, axis=2) * w[..., None], axis=2).astype(q.dtype)


def diff_attention(q, k, v, lam, subln_g, bias_tab, lambda_init):
    b, s, h = q.shape[:3]
    dh = q.shape[-1]
    n_qb = s // DIFF_Q_BLOCK
    lamf = lam.astype(jnp.float32)
    lam_full = (jnp.exp(jnp.sum(lamf[0] * lamf[1])) - jnp.exp(jnp.sum(lamf[2] * lamf[3]))
                + lambda_init)
    kpos = jnp.arange(s, dtype=jnp.int32)
    vf = v.astype(jnp.float32)
    q_blocks = jnp.swapaxes(q.reshape(b, n_qb, DIFF_Q_BLOCK, h, 2, dh), 0, 1)
    starts = jnp.arange(n_qb, dtype=jnp.int32) * DIFF_Q_BLOCK

    def block(args):
        q_blk, start = args
        qpos = start + jnp.arange(DIFF_Q_BLOCK, dtype=jnp.int32)
        bias = bias_tab[rel_bucket(kpos[None, :] - qpos[:, None])].astype(jnp.float32)
        logits = jnp.einsum('bqhmd,bkhmd->bhmqk', q_blk, k,
                            preferred_element_type=jnp.float32) * (dh ** -0.5)
        logits = logits + bias.transpose(2, 0, 1)[:, None]
        p = jax.nn.softmax(logits, axis=-1)
        attn = p[:, :, 0] - lam_full * p[:, :, 1]
        return jnp.einsum('bhqk,bkhe->bqhe', attn, vf)

    o = lax.map(block, (q_blocks, starts))
    o = jnp.swapaxes(o, 0, 1).reshape(b, s, h, DIFF_V_DIM)
    return (rms_norm(o, subln_g) * (1.0 - lambda_init)).astype(q.dtype)


def peer_ffn(h, wq, subkeys, u, v):
    b, s, d = h.shape
    q = (h @ wq).reshape(b, s, PEER_HEADS, 2, PEER_QDIM // 2)
    scores = jnp.einsum('bshpc,pkc->bshpk', q, subkeys, preferred_element_type=jnp.float32)
    top_s, top_i = lax.top_k(scores, PEER_TOPK)
    cand_s = top_s[..., 0, :, None] + top_s[..., 1, None, :]
    cand_id = top_i[..., 0, :, None] * PEER_NKEYS + top_i[..., 1, None, :]
    best_s, best_j = lax.top_k(cand_s.reshape(b, s, PEER_HEADS, PEER_TOPK * PEER_TOPK), PEER_TOPK)
    ids = jnp.take_along_axis(cand_id.reshape(b, s, PEER_HEADS, PEER_TOPK * PEER_TOPK), best_j, axis=-1)
    gates = jax.nn.softmax(best_s, axis=-1)
    n_chunks = (b * s) // PEER_CHUNK
    xs = (h.reshape(n_chunks, PEER_CHUNK, d),
          ids.reshape(n_chunks, PEER_CHUNK, PEER_HEADS * PEER_TOPK),
          gates.reshape(n_chunks, PEER_CHUNK, PEER_HEADS * PEER_TOPK))

    def chunk(args):
        xc, ic, gc = args
        pre = jnp.einsum('td,ted->te', xc, u[ic], preferred_element_type=jnp.float32)
        act = jax.nn.gelu(pre, approximate=False) * gc
        return jnp.einsum('te,ted->td', act, v[ic].astype(jnp.float32))

    out = lax.map(chunk, xs)
    return out.reshape(b, s, d).astype(h.dtype)


def setup_inputs(seed: int = 0) -> dict:
    key = jax.random.key(seed)
    ks = jax.random.split(key, 17)

    def nrm(k, shape, scale):
        return jax.random.normal(k, shape, jnp.float32) * scale

    return {
        'x': nrm(ks[0], (BATCH, SEQ, D_MODEL), 1.0),
        'rel_bias': nrm(ks[1], (REL_BUCKETS, REL_HEADS), 0.2),
        'final_norm_g': 1.0 + nrm(ks[2], (D_MODEL,), 0.02),
        'mix_norm_g': 1.0 + nrm(ks[3], (DEPTH, D_MODEL), 0.02),
        'w_in': nrm(ks[4], (DEPTH, D_MODEL, IN_WIDTH), D_MODEL ** -0.5),
        'b_gate': nrm(ks[5], (DEPTH, N_BRANCHES * D_MODEL), 0.02),
        'w_up_a': nrm(ks[6], (DEPTH, FNET_WIDTH, D_MODEL), FNET_WIDTH ** -0.5),
        'w_up_b': nrm(ks[7], (DEPTH, DIL_OUT_WIDTH, D_MODEL), DIL_OUT_WIDTH ** -0.5),
        'w_up_c': nrm(ks[8], (DEPTH, DIFF_V_WIDTH, D_MODEL), DIFF_V_WIDTH ** -0.5),
        'diff_lambda': nrm(ks[9], (DEPTH, 4, HEAD_DIM), 0.1),
        'diff_subln_g': 1.0 + nrm(ks[10], (DEPTH, DIFF_V_DIM), 0.02),
        'w_o': nrm(ks[11], (DEPTH, D_MODEL, D_MODEL), D_MODEL ** -0.5),
        'ffn_norm_g': 1.0 + nrm(ks[12], (DEPTH, D_MODEL), 0.02),
        'peer_wq': nrm(ks[13], (DEPTH, D_MODEL, PEER_HEADS * PEER_QDIM), D_MODEL ** -0.5),
        'peer_subkeys': nrm(ks[14], (DEPTH, 2, PEER_NKEYS, PEER_QDIM // 2), (PEER_QDIM // 2) ** -0.5),
        'peer_u': nrm(ks[15], (DEPTH, PEER_EXPERTS, D_MODEL), D_MODEL ** -0.5),
        'peer_v': nrm(ks[16], (DEPTH, PEER_EXPERTS, D_MODEL), PEER_TOPK ** -0.5),
    }


def reference(x, rel_bias, final_norm_g, mix_norm_g, w_in, b_gate, w_up_a, w_up_b, w_up_c,
              diff_lambda, diff_subln_g, w_o, ffn_norm_g, peer_wq, peer_subkeys, peer_u, peer_v):
    b, s, d = x.shape
    dil_bias = rel_bias[:, :DIL_GROUPS * DIL_HEADS]
    diff_bias = rel_bias[:, DIL_GROUPS * DIL_HEADS:]
    for layer in range(DEPTH):
        h = rms_norm(x, mix_norm_g[layer])
        proj = h @ w_in[layer]
        p_fnet = proj[..., COL_FNET:COL_DIL]
        p_dil = proj[..., COL_DIL:COL_DIFF].reshape(b, s, 3, DIL_GROUPS, DIL_HEADS, HEAD_DIM)
        p_diff = proj[..., COL_DIFF:COL_GATE]
        dq = p_diff[..., :DIFF_QK_WIDTH].reshape(b, s, DIFF_HEADS, 2, HEAD_DIM)
        dk = p_diff[..., DIFF_QK_WIDTH:2 * DIFF_QK_WIDTH].reshape(b, s, DIFF_HEADS, 2, HEAD_DIM)
        dv = p_diff[..., 2 * DIFF_QK_WIDTH:].reshape(b, s, DIFF_HEADS, DIFF_V_DIM)
        gates = jax.nn.sigmoid((proj[..., COL_GATE:] + b_gate[layer]).astype(jnp.float32))
        gates = gates.reshape(b, s, N_BRANCHES, d)

        y_a = fourier_mixer(p_fnet) @ w_up_a[layer]
        o_b = dilated_mixer(p_dil[:, :, 0], p_dil[:, :, 1], p_dil[:, :, 2], dil_bias)
        y_b = o_b.reshape(b, s, DIL_OUT_WIDTH) @ w_up_b[layer]
        lambda_init = 0.8 - 0.6 * math.exp(-0.3 * layer)
        o_c = diff_attention(dq, dk, dv, diff_lambda[layer], diff_subln_g[layer], diff_bias, lambda_init)
        y_c = o_c.reshape(b, s, DIFF_V_WIDTH) @ w_up_c[layer]

        merged = gates[:, :, 0] * y_a + gates[:, :, 1] * y_b + gates[:, :, 2] * y_c
        x = x + merged.astype(x.dtype) @ w_o[layer]
        h = rms_norm(x, ffn_norm_g[layer])
        x = x + peer_ffn(h, peer_wq[layer], peer_subkeys[layer], peer_u[layer], peer_v[layer])
    return rms_norm(x, final_norm_g)
```

```python
import math
from contextlib import ExitStack

import numpy as np
import concourse.bass as bass
import concourse.mybir as mybir
from concourse.bass_utils import run_bass_kernel_spmd

F32 = mybir.dt.float32
BF16 = mybir.dt.bfloat16
I32 = mybir.dt.int32
U32 = mybir.dt.uint32
AF = mybir.ActivationFunctionType
ALU = mybir.AluOpType
AX = mybir.AxisListType

D = 2048
SEQ = 4096
TOK = 2048
NT = TOK // 128
INW = 14336
EPS = 1e-6
SAME_SYNC = True
DEBUG_OUT = False
DEBUG_STOP = None


class Sem:
    def __init__(self, h):
        self.h = h
        self.cnt = 0


class Buf:
    def __init__(self, name, t, space):
        self.name = name
        self.t = t
        self.space = space
        self.w = {}
        self.r = {}
        self.sem = None

    def __getitem__(self, idx):
        return self.t[idx]

    def ap(self):
        return self.t.ap() if self.space == "dram" else self.t[:]


class Ctx:
    def __init__(self, nc):
        self.nc = nc
        self.gs = ExitStack()
        self.eng = {"pe": nc.tensor, "act": nc.scalar, "dve": nc.vector,
                    "pool": nc.gpsimd, "sp": nc.sync}
        self.esem = {}
        for k in self.eng:
            self.esem[k] = Sem(self.gs.enter_context(nc.semaphore("es_" + k)))
        self.waited = {k: {} for k in self.eng}
        self.free_sems = [Sem(self.gs.enter_context(nc.semaphore("ds_%d" % i))) for i in range(72)]
        self.all_sems = list(self.free_sems)
        self.phase_stack = None
        self.phase_bufs = []
        self.dram_bufs = []
        self.uid = 0

    def begin_phase(self):
        self.scopes = [(ExitStack(), [])]
        self.phase_stack, self.phase_bufs = self.scopes[-1]

    def push_scope(self):
        self.scopes.append((ExitStack(), []))
        self.phase_stack, self.phase_bufs = self.scopes[-1]

    def pop_scope(self):
        self.barrier()
        st, bufs = self.scopes.pop()
        for b in bufs:
            if b.sem is not None:
                self.free_sems.append(b.sem)
                b.sem = None
        st.close()
        if self.scopes:
            self.phase_stack, self.phase_bufs = self.scopes[-1]
        else:
            self.phase_stack, self.phase_bufs = None, []

    def end_phase(self):
        while self.scopes:
            self.pop_scope()

    def sb(self, name, shape, dtype):
        self.uid += 1
        t = self.phase_stack.enter_context(self.nc.sbuf_tensor("%s_%d" % (name, self.uid), list(shape), dtype))
        b = Buf(name, t, "sb")
        self.phase_bufs.append(b)
        return b

    def ps(self, name, shape, dtype=F32):
        self.uid += 1
        t = self.phase_stack.enter_context(self.nc.psum_tensor("%s_%d" % (name, self.uid), list(shape), dtype))
        b = Buf(name, t, "ps")
        self.phase_bufs.append(b)
        return b

    def sbg(self, name, shape, dtype):
        self.uid += 1
        t = self.gs.enter_context(self.nc.sbuf_tensor("%s_%d" % (name, self.uid), list(shape), dtype))
        return Buf(name, t, "sb")

    def dram(self, name, shape, dtype, kind="Internal"):
        if kind == "Internal":
            t = self.nc.dram_tensor(name, list(shape), dtype)
        else:
            t = self.nc.dram_tensor(name, list(shape), dtype, kind=kind)
        b = Buf(name, t, "dram")
        self.dram_bufs.append(b)
        return b

    def _need(self, e, events):
        eng = self.eng[e]
        wd = self.waited[e]
        for ev in events:
            sem, val = ev
            if sem is self.esem.get(e):
                if e == "pe" or not SAME_SYNC:
                    continue
            if wd.get(id(sem), 0) >= val:
                continue
            eng.wait_ge(sem.h, val)
            wd[id(sem)] = val

    @staticmethod
    def _resolve(evs):
        out = []
        for ev in evs:
            if ev[0] == "dma":
                out.append((ev[1], ev[1].cnt))
            else:
                out.append(ev)
        return out

    def op(self, e, fn, reads=(), writes=(), lax=None):
        evs = []
        lax_ids = set(id(b) for b in lax) if lax else ()
        own = self.esem[e]
        for b in reads:
            evs.extend(b.w.values())
        for b in writes:
            wr = list(b.w.values()) + list(b.r.values())
            if id(b) in lax_ids:
                wr = [ev for ev in wr if ev[0] is not own]
            evs.extend(wr)
        evs = self._resolve(evs)
        self._need(e, evs)
        inst = fn(self.eng[e])
        s = self.esem[e]
        s.cnt += 1
        inst.then_inc(s.h, 1)
        me = (s, s.cnt)
        for b in reads:
            b.r[e] = me
        for b in writes:
            b.w = {e: me}
            b.r = {}
        return inst

    def dma(self, out_ap, in_ap, dst, src, q="sp", fn=None, slow=False, extra_reads=()):
        owner = dst if dst.space == "sb" else (src if src.space == "sb" else dst)
        if owner.sem is None:
            owner.sem = self.free_sems.pop()
        evs = list(src.w.values())
        for b in extra_reads:
            evs.extend(b.w.values())
        if dst.space != "dram":
            evs.extend(dst.w.values())
            evs.extend(dst.r.values())
        self._need(q, self._resolve(evs))
        if fn is None:
            if slow:
                inst = self.eng[q].dma_start(out=out_ap, in_=in_ap, allow_slow_non_contiguous=True)
            else:
                inst = self.eng[q].dma_start(out=out_ap, in_=in_ap)
        else:
            inst = fn(self.eng[q])
        owner.sem.cnt += 16
        inst.then_inc(owner.sem.h, 16)
        ev = ("dma", owner.sem)
        key = id(owner.sem)
        src.r[key] = ev
        for b in extra_reads:
            b.r[key] = ev
        if dst.space == "dram":
            dst.w[key] = ev
        else:
            dst.w = {key: ev}
            dst.r = {}
        return inst

    def allgather(self, src, src_ap, dst, dst_ap):
        evs = list(src.w.values()) + list(dst.w.values()) + list(dst.r.values())
        self._need("pool", self._resolve(evs))
        if dst.sem is None:
            dst.sem = self.free_sems.pop()
        inst = self.eng["pool"].collective_compute("AllGather", ALU.bypass, replica_groups=[[0, 1], [2, 3], [4, 5], [6, 7]],
                                                   ins=[src_ap.opt()], outs=[dst_ap.opt()])
        dst.sem.cnt += 1
        inst.then_inc(dst.sem.h)
        ev = (dst.sem, dst.sem.cnt)
        dst.w[id(dst.sem)] = ev
        src.r[id(dst.sem)] = ev
        return inst

    def barrier(self):
        evs = [(s, s.cnt) for s in self.esem.values()]
        evs += [(s, s.cnt) for s in self.all_sems if s.cnt > 0]
        save = SAME_SYNC
        for e in self.eng:
            wd = self.waited[e]
            for sem, val in evs:
                if wd.get(id(sem), 0) >= val:
                    continue
                self.eng[e].wait_ge(sem.h, val)
                wd[id(sem)] = val
        for b in self.dram_bufs:
            b.w = {}
            b.r = {}

    def finish(self):
        self.barrier()
        self.gs.close()


def dram_ap(buf, offset, pattern):
    return bass.AP(tensor=buf.t, offset=offset, ap=[list(p) for p in pattern])


def load_consts(cx, T):
    G = {}
    G["ident_bf"] = cx.sbg("identbf", [128, 128], BF16)
    cx.dma(G["ident_bf"][:], T["c_ident_bf"].t.ap(), G["ident_bf"], T["c_ident_bf"])
    G["ident_f"] = cx.sbg("identf", [128, 128], F32)
    cx.dma(G["ident_f"][:], T["c_ident_f"].t.ap(), G["ident_f"], T["c_ident_f"])
    G["J"] = cx.sbg("Jf", [128, 128], F32)
    cx.dma(G["J"][:], T["c_J"].t.ap(), G["J"], T["c_J"])
    G["iota16"] = cx.sbg("iota16", [128, 16], F32)
    cx.dma(G["iota16"][:], T["c_iota16"].t.ap(), G["iota16"], T["c_iota16"])
    if "c_half" in T:
        G["half"] = cx.sbg("halfm", [128, 2], F32)
        cx.dma(G["half"][:], T["c_half"].t.ap(), G["half"], T["c_half"])
    G["ones_bf"] = cx.sbg("onesbf", [128, 2], BF16)
    cx.op("dve", lambda e: e.memset(G["ones_bf"][:], 1.0), writes=[G["ones_bf"]])
    return G


def rmsnorm_to_hT(cx, x_dram, g_dram_row, hT, ident_bf, consts, xf_keep=None):
    nc = cx.nc
    cx.push_scope()
    gb = cx.sb("gb", [128, D], F32)
    gbuf, goff = g_dram_row
    cx.dma(gb[:], dram_ap(gbuf, goff, [[0, 128], [1, D]]), gb, gbuf)
    xt = [cx.sb("xt%d" % i, [128, D], F32) for i in range(2)]
    junk = cx.sb("junk", [128, D], BF16)
    xs = [cx.sb("xs%d" % i, [128, D], BF16) for i in range(2)]
    st = [cx.sb("st%d" % i, [128, 4], F32) for i in range(2)]
    tp = [cx.ps("tp%d" % i, [128, 8, 128], BF16) for i in range(2)]
    for tt in range(NT):
        x_t = xt[tt % 2]
        s_t = st[tt % 2]
        x_s = xs[tt % 2]
        cx.dma(x_t[:], x_dram.t.ap()[tt * 128:(tt + 1) * 128, :], x_t, x_dram)
        cx.op("act", lambda e: e.activation(out=junk[:], in_=x_t[:], func=AF.Square, accum_out=s_t[:, 0:1]),
              reads=[x_t], writes=[junk, s_t])
        cx.op("dve", lambda e: e.tensor_scalar(out=s_t[:, 1:2], in0=s_t[:, 0:1], scalar1=1.0 / D, scalar2=EPS,
                                               op0=ALU.mult, op1=ALU.add), reads=[s_t], writes=[s_t])
        cx.op("act", lambda e: e.sqrt(out=s_t[:, 2:3], in_=s_t[:, 1:2]), reads=[s_t], writes=[s_t])
        cx.op("dve", lambda e: e.reciprocal(out=s_t[:, 3:4], in_=s_t[:, 2:3]), reads=[s_t], writes=[s_t])
        cx.op("dve", lambda e: e.scalar_tensor_tensor(out=x_s[:], in0=x_t[:], scalar=s_t[:, 3:4], in1=gb[:],
                                                      op0=ALU.mult, op1=ALU.mult), reads=[x_t, s_t, gb], writes=[x_s])
        for half in range(2):
            p = tp[half]
            for j in range(8):
                k = half * 8 + j
                cx.op("pe", lambda e: e.transpose(out=p[:, j, :], in_=x_s[:, k * 128:(k + 1) * 128], identity=ident_bf[:]),
                      reads=[x_s, ident_bf], writes=[p])
            eng = "act" if half == 0 else "dve"
            if eng == "act":
                cx.op("act", lambda e: e.copy(out=hT[:, half * 8:(half + 1) * 8, tt * 128:(tt + 1) * 128], in_=p[:]),
                      reads=[p], writes=[hT])
            else:
                cx.op("dve", lambda e: e.tensor_copy(out=hT[:, half * 8:(half + 1) * 8, tt * 128:(tt + 1) * 128], in_=p[:]),
                      reads=[p], writes=[hT])
    cx.pop_scope()


def load_w_bf16(cx, wbuf, row0, col0, ncols, stg, wbf, conv_eng):
    rowlen = wbuf.rowlen
    src = dram_ap(wbuf, wbuf.base + row0 * rowlen + col0, [[rowlen, 128], [128 * rowlen, 16], [1, ncols]])
    cx.dma(stg[:, :, 0:ncols], src, stg, wbuf)
    if conv_eng == "pool":
        cx.op("act", lambda e: e.copy(out=wbf[:, 0:8, 0:ncols], in_=stg[:, 0:8, 0:ncols]), reads=[stg], writes=[wbf])
        cx.op("dve", lambda e: e.tensor_copy(out=wbf[:, 8:16, 0:ncols], in_=stg[:, 8:16, 0:ncols]), reads=[stg], writes=[wbf])
    elif conv_eng == "dve":
        cx.op("dve", lambda e: e.tensor_copy(out=wbf[:, :, 0:ncols], in_=stg[:, :, 0:ncols]), reads=[stg], writes=[wbf])
    else:
        cx.op("act", lambda e: e.copy(out=wbf[:, :, 0:ncols], in_=stg[:, :, 0:ncols]), reads=[stg], writes=[wbf])


AG_AFTER = {0: [("k", 0)], 4: [("k", 1)], 5: [("k", 2)], 6: [("k", 3)], 12: [("k", 4)], 13: [("k", 5)],
            15: [("v", c) for c in range(8)]}


def conv_begin(cx, T, layer):
    T["conv"] = {"layer": layer, "next": 0, "total": 256,
                 "cin": [cx.sb("cvi%d" % i, [128, D], F32) for i in range(3)],
                 "cout": [cx.sb("cvo%d" % i, [128, D], BF16) for i in range(3)]}


def conv_step(cx, T, ntiles):
    st = T.get("conv")
    if st is None:
        return
    wl = T["wl"][st["layer"]]

    def finish_tile(t):
        dstn = "uv16"
        row0 = wl * 16384 + (t % 128) * 128
        ci, co = st["cin"][t % 3], st["cout"][t % 3]
        if t % 2 == 0:
            cx.op("act", lambda e: e.copy(out=co[:], in_=ci[:]), reads=[ci], writes=[co])
        else:
            cx.op("dve", lambda e: e.tensor_copy(out=co[:], in_=ci[:]), reads=[ci], writes=[co])
        cx.dma(dram_ap(T[dstn], row0 * 2 * D + (0 if t < 128 else D), [[2 * D, 128], [1, D]]), co[:], T[dstn], co, q="pool")

    for _ in range(ntiles):
        t = st["next"]
        if t > st["total"]:
            return
        st["next"] = t + 1
        if t < st["total"]:
            tab = "peer_u" if t < 128 else "peer_v"
            row0 = wl * 16384 + (t % 128) * 128
            ci = st["cin"][t % 3]
            cx.dma(ci[:], dram_ap(T[tab], row0 * D, [[D, 128], [1, D]]), ci, T[tab])
        if t >= 1:
            finish_tile(t - 1)


def phase_A(cx, layer, x_dram, T):
    cx.begin_phase()
    ident_bf = T["G"]["ident_bf"]
    hT = cx.sb("hT", [128, 16, TOK], BF16)
    rmsnorm_to_hT(cx, x_dram, (T["mix_norm_g"], T["wl"][layer] * D), hT, ident_bf, T)
    bg = cx.sb("bgate", [128, 48], F32)
    cx.dma(bg[:], dram_ap(T["b_gate"], T["wl"][layer] * 6144, [[1, 128], [128, 48]]), bg, T["b_gate"], slow=True)
    stg = [cx.sb("wstg%d" % i, [128, 16, 512], F32) for i in range(2)]
    wbf = [cx.sb("wbf%d" % i, [128, 16, 512], BF16) for i in range(2)]
    if "uv16" in T:
        conv_begin(cx, T, layer)
    pss = [cx.ps("psA%d" % i, [128, 512], F32) for i in range(4)]
    obf = [cx.sb("obf%d" % i, [128, 512], BF16) for i in range(3)]
    of32 = [cx.sb("of32%d" % i, [128, 512], F32) for i in range(3)]
    w_in = T["w_in"]
    w_in.rowlen = INW
    w_in.base = T["wl"][layer] * D * INW
    cnt = {"ps": 0, "o": 0, "ev": 0}
    qscale = 128 ** -0.5
    NCB = INW // 512

    def w_dma(cb):
        src = dram_ap(w_in, w_in.base + cb * 512, [[INW, 128], [128 * INW, 16], [1, 512]])
        cx.dma(stg[cb % 2][:], src, stg[cb % 2], w_in)

    def w_cast(cb):
        s2, w2 = stg[cb % 2], wbf[cb % 2]
        cx.op("act", lambda e: e.copy(out=w2[:, 0:8, :], in_=s2[:, 0:8, :]), reads=[s2], writes=[w2])
        cx.op("dve", lambda e: e.tensor_copy(out=w2[:, 8:16, :], in_=s2[:, 8:16, :]), reads=[s2], writes=[w2])

    w_dma(0)
    w_cast(0)
    for cb in range(NCB):
        c0 = cb * 512
        s_, w_ = stg[cb % 2], wbf[cb % 2]
        if cb + 1 < NCB:
            w_dma(cb + 1)
        if c0 < 512:
            kind, dest, r0, scale = "f", T["kfT"], c0, 1.0
        elif c0 < 2048:
            kind, dest, r0, scale = "f", T["qdT"], c0 - 512, qscale
        elif c0 < 3584:
            kind, dest, r0, scale = "f", T["kfT"], 512 + (c0 - 2048), 1.0
        elif c0 < 5120:
            kind, dest, r0, scale = "v", T["vtok"], c0 - 3584, 1.0
        elif c0 < 6144:
            kind, dest, r0, scale = "f", T["qcT"], c0 - 5120, qscale
        elif c0 < 7168:
            kind, dest, r0, scale = "f", T["kfT"], 2048 + (c0 - 6144), 1.0
        elif c0 < 8192:
            kind, dest, r0, scale = "v", T["vtok"], 1536 + (c0 - 7168), 1.0
        else:
            kind, dest, r0, scale = "g", T["gatesT"], c0 - 8192, 1.0
        step = 0
        if kind in ("f", "g"):
            for m in range(4):
                for tb in range(4):
                    if step < 10:
                        conv_step(cx, T, 1)
                    if step == 8 and cb + 1 < NCB:
                        w_cast(cb + 1)
                    step += 1
                    p = pss[cnt["ps"] % 4]
                    cnt["ps"] += 1
                    for k in range(16):
                        cx.op("pe", lambda e: e.matmul(out=p[:], lhsT=w_[:, k, m * 128:(m + 1) * 128],
                                                       rhs=hT[:, k, tb * 512:(tb + 1) * 512],
                                                       start=(k == 0), stop=(k == 15)),
                              reads=[w_, hT], writes=[p])
                    row = r0 + m * 128
                    if kind == "f":
                        o = obf[cnt["o"] % 3]
                        cnt["o"] += 1
                        if cnt["ev"] % 2 == 0:
                            cx.op("act", lambda e: e.activation(out=o[:], in_=p[:], func=AF.Copy, scale=scale),
                                  reads=[p], writes=[o])
                        else:
                            cx.op("dve", lambda e: e.tensor_scalar(out=o[:], in0=p[:], scalar1=scale, scalar2=None,
                                                                   op0=ALU.mult), reads=[p], writes=[o])
                        sq = "act" if cnt["ev"] % 2 == 0 else "pool"
                        cnt["ev"] += 1
                        cx.dma(dest.t.ap()[row:row + 128, tb * 512:(tb + 1) * 512], o[:], dest, o, q=sq)
                    else:
                        o = of32[cnt["o"] % 3]
                        cnt["o"] += 1
                        gt = (c0 - 8192) // 128 + m
                        cx.op("act", lambda e: e.activation(out=o[:], in_=p[:], func=AF.Sigmoid,
                                                            bias=bg[:, gt:gt + 1], scale=1.0),
                              reads=[p, bg], writes=[o])
                        cx.dma(dest.t.ap()[row:row + 128, tb * 512:(tb + 1) * 512], o[:], dest, o, q="act")
        else:
            for tt in range(NT):
                if step < 10:
                    conv_step(cx, T, 1)
                if step == 8 and cb + 1 < NCB:
                    w_cast(cb + 1)
                step += 1
                p = pss[cnt["ps"] % 4]
                cnt["ps"] += 1
                for k in range(16):
                    cx.op("pe", lambda e: e.matmul(out=p[:], lhsT=hT[:, k, tt * 128:(tt + 1) * 128],
                                                   rhs=w_[:, k, :], start=(k == 0), stop=(k == 15)),
                          reads=[w_, hT], writes=[p])
                o = obf[cnt["o"] % 3]
                cnt["o"] += 1
                if cnt["ev"] % 2 == 0:
                    cx.op("act", lambda e: e.copy(out=o[:], in_=p[:]), reads=[p], writes=[o])
                else:
                    cx.op("dve", lambda e: e.tensor_copy(out=o[:], in_=p[:]), reads=[p], writes=[o])
                sq = "act" if cnt["ev"] % 2 == 0 else "pool"
                cnt["ev"] += 1
                cx.dma(dest.t.ap()[tt * 128:(tt + 1) * 128, r0:r0 + 512], o[:], dest, o, q=sq)
        if "kf_g" in T:
            for kind_, c in AG_AFTER.get(cb, ()):
                if kind_ == "k":
                    cx.allgather(T["kfT"], T["kfT"].t.ap()[c * 512:(c + 1) * 512, :], T["kf_g"],
                                 T["kf_g"].t.ap()[c * 1024:(c + 1) * 1024, :])
                else:
                    cx.allgather(T["vtok"], T["vtok"].t.ap()[c * 256:(c + 1) * 256, :], T["v_g"],
                                 T["v_g"].t.ap()[c * 512:(c + 1) * 512, :])
            T["ag_done"] = True
    conv_step(cx, T, 256)
    T["conv"] = None
    cx.end_phase()


DIFF_OFF = 1920
DIFF_W = 3968
DIL = ((1, 64), (4, 64), (16, 64))


def phase_bias(cx, T):
    G = T["G"]
    cx.begin_phase()
    rb = cx.sb("rb33", [33, 16], F32)
    cx.op("dve", lambda e: e.memset(rb[:], -30000.0), writes=[rb])
    cx.dma(rb[0:32, :], T["rel_bias"].t.ap(), rb, T["rel_bias"])
    ohd = cx.sb("ohd", [32, 2, 4096], F32)
    cx.dma(ohd[:], T["c_oh_diff"].t.ap(), ohd, T["c_oh_diff"])
    ohl = cx.sb("ohl", [33, 3, 512], F32)
    cx.dma(ohl[:], T["c_oh_dil"].t.ap(), ohl, T["c_oh_dil"])
    ps = [cx.ps("pb%d" % i, [128, 512], F32) for i in range(2)]
    rows = cx.sb("rows", [16, 512], F32)
    n = 0
    for s_ in range(2):
        for c in range(8):
            p = ps[n % 2]
            n += 1
            cx.op("pe", lambda e: e.matmul(out=p[0:4, :], lhsT=rb[0:32, 12:16], rhs=ohd[:, s_, c * 512:(c + 1) * 512],
                                           start=True, stop=True), reads=[rb, ohd], writes=[p])
            cx.op("act", lambda e: e.copy(out=rows[0:4, :], in_=p[0:4, :]), reads=[p], writes=[rows])
            cx.dma(dram_ap(T["rb_diff"], s_ * 4096 + c * 512, [[8192, 4], [1, 512]]), rows[0:4, :], T["rb_diff"], rows)
    for g in range(3):
        p = ps[n % 2]
        n += 1
        cx.op("pe", lambda e: e.matmul(out=p[0:12, :], lhsT=rb[0:33, 0:12], rhs=ohl[:, g, :], start=True, stop=True),
              reads=[rb, ohl], writes=[p])
        cx.op("act", lambda e: e.copy(out=rows[0:12, :], in_=p[0:12, :]), reads=[p], writes=[rows])
        cx.dma(dram_ap(T["rows_dil"], g * 12 * 512, [[512, 12], [1, 512]]), rows[0:12, :], T["rows_dil"], rows)
    hk = [cx.sb("hk%d" % i, [128, DIFF_W], F32) for i in range(2)]
    ob = [cx.sb("ob%d" % i, [128, 512], F32) for i in range(2)]
    m = 0
    for h in range(4):
        for s_ in range(2):
            hb = hk[m % 2]
            m += 1
            cx.dma(hb[:], dram_ap(T["rb_diff"], h * 8192 + s_ * 4096, [[1, 128], [1, DIFF_W]]), hb, T["rb_diff"])
            for c in range(0, DIFF_W, 512):
                w = min(512, DIFF_W - c)
                p = ps[n % 2]
                o = ob[n % 2]
                n += 1
                cx.op("pe", lambda e: e.matmul(out=p[:, 0:w], lhsT=G["J"][:], rhs=hb[:, c:c + w], start=True, stop=True),
                      reads=[G["J"], hb], writes=[p])
                cx.op("act", lambda e: e.copy(out=o[:, 0:w], in_=p[:, 0:w]), reads=[p], writes=[o])
                cx.dma(dram_ap(T["strip_diff"], ((h * 2 + s_) * 128) * DIFF_W + c, [[DIFF_W, 128], [1, w]]), o[:, 0:w],
                       T["strip_diff"], o)
    hk2 = [cx.sb("hk2%d" % i, [128, 2, 128], F32) for i in range(2)]
    for g in range(3):
        for hh in range(4):
            hd = g * 4 + hh
            hb = hk2[m % 2]
            m += 1
            cx.dma(hb[:], dram_ap(T["rows_dil"], (g * 12 + hd) * 512, [[1, 128], [256, 2], [1, 128]]), hb, T["rows_dil"])
            p = ps[n % 2]
            o = ob[n % 2]
            n += 1
            cx.op("pe", lambda e: e.matmul(out=p[:, 0:256], lhsT=G["J"][:], rhs=hb[:].rearrange("p a b -> p (a b)"),
                                           start=True, stop=True), reads=[G["J"], hb], writes=[p])
            cx.op("act", lambda e: e.copy(out=o[:, 0:256], in_=p[:, 0:256]), reads=[p], writes=[o])
            cx.dma(dram_ap(T["tm_dil"], hd * 128 * 256, [[256, 128], [1, 256]]), o[:, 0:256], T["tm_dil"], o)
    cx.end_phase()


def phase_fnet(cx, T):
    cx.begin_phase()
    zT = cx.sb("zT", [128, 4, SEQ], BF16)
    if "kf_g" in T:
        for r in range(2):
            cx.dma(zT[:, :, r * TOK:(r + 1) * TOK], dram_ap(T["kf_g"], (r * 512) * TOK, [[TOK, 128], [128 * TOK, 4], [1, TOK]]),
                   zT, T["kf_g"])
    else:
        cx.dma(zT[:], dram_ap(T["fz"], 0, [[SEQ, 128], [128 * SEQ, 4], [1, SEQ]]), zT, T["fz"])
    dc = cx.sb("dftc", [128, 256], BF16)
    cx.dma(dc[:], T["c_dftc"].t.ap(), dc, T["c_dftc"])
    zz = [cx.sb("zz%d" % g, [128, 32, 256], BF16) for g in range(4)]
    ps = [cx.ps("pf%d" % i, [128, 512], F32) for i in range(4)]
    n = 0
    for g in range(4):
        for s2 in range(16):
            p = ps[n % 4]
            for u in range(2):
                st = s2 * 2 + u
                cx.op("pe", lambda e: e.matmul(out=p[:, u * 256:(u + 1) * 256], lhsT=zT[:, g, st * 128:(st + 1) * 128],
                                               rhs=dc[:], start=True, stop=True), reads=[zT, dc], writes=[p])
            if n % 2 == 0:
                cx.op("act", lambda e: e.copy(out=zz[g][:, s2 * 2:s2 * 2 + 2, :].rearrange("p a b -> p (a b)"), in_=p[:]),
                      reads=[p], writes=[zz[g]])
            else:
                cx.op("dve", lambda e: e.tensor_copy(out=zz[g][:, s2 * 2:s2 * 2 + 2, :].rearrange("p a b -> p (a b)"), in_=p[:]),
                      reads=[p], writes=[zz[g]])
            n += 1
    cs = cx.sb("csb", [128, 2, 32, 512], BF16)
    ob = [cx.sb("fo%d" % i, [128, 512], BF16) for i in range(2)]
    sc = 1.0 / math.sqrt(SEQ * 128.0)
    for kb in range(4):
        for t in range(2):
            cx.dma(cs[:, t, :, :], dram_ap(T["c_dfts"], t * SEQ * TOK + kb * 512, [[TOK, 128], [128 * TOK, 32], [1, 512]]),
                   cs, T["c_dfts"])
        for g in range(4):
            p = ps[n % 4]
            o = ob[n % 2]
            n += 1
            i = 0
            for st in range(32):
                for t in range(2):
                    cx.op("pe", lambda e: e.matmul(out=p[:], lhsT=zz[g][:, st, t * 128:(t + 1) * 128], rhs=cs[:, t, st, :],
                                                   start=(i == 0), stop=(i == 63)), reads=[zz[g], cs], writes=[p])
                    i += 1
            cx.op("act", lambda e: e.activation(out=o[:], in_=p[:], func=AF.Copy, scale=sc), reads=[p], writes=[o])
            cx.dma(T["mixT"].t.ap()[g * 128:(g + 1) * 128, kb * 512:(kb + 1) * 512], o[:], T["mixT"], o)
    cx.end_phase()


def phase_dil(cx, T):
    G = T["G"]
    cx.begin_phase()
    RS = 12 * 129
    qt = [cx.sb("dq%d" % i, [128, TOK], BF16) for i in range(2)]
    kt = [cx.sb("dk%d" % i, [128, 3 * TOK], BF16) for i in range(2)]
    tm = [cx.sb("dtm%d" % i, [128, 2, 128], F32) for i in range(2)]
    vt = [cx.sb("dv%d" % i, [128, 2, 129], BF16) for i in range(4)]
    sps = [cx.ps("dsp%d" % i, [128, 2, 128], F32) for i in range(2)]
    ops = [cx.ps("dop%d" % i, [128, 129], F32) for i in range(2)]
    tmp = [cx.sb("dtmp%d" % i, [128, 2, 128], F32) for i in range(2)]
    pb = [cx.sb("dpb%d" % i, [128, 2, 128], BF16) for i in range(2)]
    osb = [cx.sb("dosb%d" % i, [128, 129], F32) for i in range(3)]
    n = 0
    for g in range(3):
        d = DIL[g][0]
        nloc = TOK // d
        for hh in range(4):
            hd = g * 4 + hh
            q_, k_, tm_ = qt[hd % 2], kt[hd % 2], tm[hd % 2]
            cx.dma(q_[:], T["qdT"].t.ap()[hd * 128:(hd + 1) * 128, :], q_, T["qdT"])
            if "kf_g" in T:
                f = 512 + hd * 128
                cx.dma(k_[:, TOK:2 * TOK], T["kfT"].t.ap()[f:f + 128, :], k_, T["kfT"])
                for r in range(2):
                    row = (f // 512) * 1024 + r * 512 + f % 512
                    dst = k_[:, 0:TOK] if r == 0 else k_[:, 2 * TOK:3 * TOK]
                    cx.dma(dst, T["kf_g"].t.ap()[row:row + 128, :], k_, T["kf_g"])
                    msk = G["half"][:, 1:2] if r == 0 else G["half"][:, 0:1]
                    cx.op("dve", lambda e: e.tensor_scalar(out=dst, in0=dst, scalar1=msk, scalar2=None, op0=ALU.mult),
                          reads=[k_, G["half"]], writes=[k_])
            else:
                cx.dma(k_[:], T["kd_ext"].t.ap()[hd * 128:(hd + 1) * 128, :], k_, T["kd_ext"])
            cx.dma(tm_[:], dram_ap(T["tm_dil"], hd * 128 * 256, [[256, 128], [128, 2], [1, 128]]), tm_, T["tm_dil"])
            blocks = [(r, blk) for r in range(d) for blk in range(nloc // 128)]

            def geom(bi):
                r, blk = blocks[bi]
                m0 = blk * 128
                row0 = TOK + (m0 - 64) * d + r
                qs = m0 * d + r
                return row0, qs

            def emit_S(bi, nn):
                row0, qs = geom(bi)
                sp = sps[nn % 2]
                qap = q_[:, qs:qs + 127 * d + 1:d] if d > 1 else q_[:, qs:qs + 128]
                for t in range(2):
                    ks = row0 + t * 128 * d
                    kap = k_[:, ks:ks + 127 * d + 1:d] if d > 1 else k_[:, ks:ks + 128]
                    cx.op("pe", lambda e: e.matmul(out=sp[:, t, :], lhsT=kap, rhs=qap, start=True, stop=True),
                          reads=[k_, q_], writes=[sp])

            emit_S(0, n)
            for bi in range(len(blocks)):
                row0, qs = geom(bi)
                v_ = vt[n % 4]
                sp, op_, tp_, p_, o_ = sps[n % 2], ops[n % 2], tmp[n % 2], pb[n % 2], osb[n % 3]
                cx.dma(v_[:], dram_ap(T["vd_ext"], row0 * RS + hd * 129, [[d * RS, 128], [128 * d * RS, 2], [1, 129]]),
                       v_, T["vd_ext"])
                cx.op("dve", lambda e: e.tensor_tensor(out=tp_[:], in0=sp[:], in1=tm_[:], op=ALU.add),
                      reads=[sp, tm_], writes=[tp_])
                cx.op("act", lambda e: e.activation(out=p_[:], in_=tp_[:], func=AF.Exp), reads=[tp_], writes=[p_])
                if bi + 1 < len(blocks):
                    emit_S(bi + 1, n + 1)
                for t in range(2):
                    cx.op("pe", lambda e: e.matmul(out=op_[:], lhsT=p_[:, t, :], rhs=v_[:, t, :], start=(t == 0), stop=(t == 1)),
                          reads=[p_, v_], writes=[op_])
                cx.op("act", lambda e: e.copy(out=o_[:], in_=op_[:]), reads=[op_], writes=[o_])
                cx.dma(dram_ap(T["numd"], g * TOK * 4 * 129 + (qs * 4 + hh) * 129, [[d * 4 * 129, 128], [1, 129]]), o_[:],
                       T["numd"], o_, q="pool")
                n += 1
    nb = [cx.sb("dnb%d" % i, [128, 3, 4 * 129], F32) for i in range(2)]
    sm = [cx.sb("dsm%d" % i, [128, 4, 129], F32) for i in range(2)]
    rd = [cx.sb("drd%d" % i, [128, 4], F32) for i in range(2)]
    on = [cx.sb("don%d" % i, [128, 4, 128], F32) for i in range(2)]
    tps = [cx.ps("dtp%d" % i, [128, 4, 128], F32) for i in range(2)]
    oT = [cx.sb("doT%d" % i, [128, 4, 128], BF16) for i in range(2)]
    for tt in range(NT):
        i = tt % 2
        cx.dma(nb[i][:], dram_ap(T["numd"], tt * 128 * 516, [[516, 128], [TOK * 516, 3], [1, 516]]), nb[i], T["numd"])
        smf = sm[i][:].rearrange("p a b -> p (a b)")
        cx.op("dve", lambda e: e.tensor_tensor(out=smf, in0=nb[i][:, 0, :], in1=nb[i][:, 1, :], op=ALU.add),
              reads=[nb[i]], writes=[sm[i]])
        cx.op("dve", lambda e: e.tensor_tensor(out=smf, in0=smf, in1=nb[i][:, 2, :], op=ALU.add),
              reads=[nb[i], sm[i]], writes=[sm[i]])
        cx.op("dve", lambda e: e.reciprocal(out=rd[i][:], in_=sm[i][:, :, 128]), reads=[sm[i]], writes=[rd[i]])
        cx.op("dve", lambda e: e.tensor_tensor(out=on[i][:], in0=sm[i][:, :, 0:128],
                                               in1=rd[i][:].unsqueeze(2).broadcast_to([128, 4, 128]), op=ALU.mult),
              reads=[sm[i], rd[i]], writes=[on[i]])
        for hh in range(4):
            cx.op("pe", lambda e: e.transpose(out=tps[i][:, hh, :], in_=on[i][:, hh, :], identity=G["ident_f"][:]),
                  reads=[on[i], G["ident_f"]], writes=[tps[i]])
        cx.op("act", lambda e: e.copy(out=oT[i][:], in_=tps[i][:]), reads=[tps[i]], writes=[oT[i]])
        cx.dma(dram_ap(T["mixT"], 512 * TOK + tt * 128, [[TOK, 128], [128 * TOK, 4], [1, 128]]), oT[i][:], T["mixT"], oT[i])
    cx.end_phase()


def phase_diff(cx, T, layer):
    G = T["G"]
    lambda_init = 0.8 - 0.6 * math.exp(-0.3 * layer)
    cx.begin_phase()
    lam = cx.sb("lam", [1, 512], F32)
    cx.dma(lam[:], dram_ap(T["diff_lambda"], T["wl"][layer] * 512, [[512, 1], [1, 512]]), lam, T["diff_lambda"])
    lj = cx.sb("lamj", [1, 128], F32)
    ls = cx.sb("lams", [1, 4], F32)
    for i in range(2):
        cx.op("dve", lambda e: e.scalar_tensor_tensor(out=lj[:], in0=lam[:, i * 256:i * 256 + 128], scalar=1.0,
                                                      in1=lam[:, i * 256 + 128:i * 256 + 256], op0=ALU.mult, op1=ALU.mult,
                                                      accum_out=ls[:, i:i + 1]), reads=[lam], writes=[lj, ls])
    cx.op("act", lambda e: e.activation(out=ls[:, 2:4], in_=ls[:, 0:2], func=AF.Exp), reads=[ls], writes=[ls])
    cx.op("dve", lambda e: e.tensor_tensor(out=ls[:, 0:1], in0=ls[:, 2:3], in1=ls[:, 3:4], op=ALU.subtract),
          reads=[ls], writes=[ls])
    cx.op("dve", lambda e: e.tensor_scalar(out=ls[:, 1:2], in0=ls[:, 0:1], scalar1=lambda_init, scalar2=-1.0,
                                           op0=ALU.add, op1=ALU.mult), reads=[ls], writes=[ls])
    ones1 = cx.sb("ones1", [1, 128], F32)
    cx.op("dve", lambda e: e.memset(ones1[:], 1.0), writes=[ones1])
    lps = cx.ps("lps", [128, 512], F32)
    cx.op("pe", lambda e: e.matmul(out=lps[:, 0:1], lhsT=ones1[:], rhs=ls[:, 1:2], start=True, stop=True),
          reads=[ones1, ls], writes=[lps])
    nlam = cx.sb("nlam", [128, 1], F32)
    cx.op("act", lambda e: e.copy(out=nlam[:], in_=lps[:, 0:1]), reads=[lps], writes=[nlam])
    gsub = cx.sb("gsub", [128, 256], F32)
    cx.dma(gsub[:], dram_ap(T["diff_subln_g"], T["wl"][layer] * 256, [[0, 128], [1, 256]]), gsub, T["diff_subln_g"])
    cx.op("act", lambda e: e.mul(out=gsub[:], in_=gsub[:], mul=1.0 - lambda_init), reads=[gsub], writes=[gsub])
    qt2 = [cx.sb("cq%d" % i, [128, 2, TOK], BF16) for i in range(2)]
    kt2 = [cx.sb("ck%d" % i, [128, 2, SEQ], BF16) for i in range(2)]
    vv2 = [cx.sb("cv%d" % i, [128, 32, 256], BF16) for i in range(2)]
    strip2 = [cx.sb("cstrip%d" % i, [128, 2, DIFF_W], F32) for i in range(2)]

    def load_head(h):
        qt, kt, vv, strip = qt2[h % 2], kt2[h % 2], vv2[h % 2], strip2[h % 2]
        cx.dma(qt[:], dram_ap(T["qcT"], h * 256 * TOK, [[TOK, 128], [128 * TOK, 2], [1, TOK]]), qt, T["qcT"])
        for m in range(2):
            f = 2048 + h * 256 + m * 128
            for r in range(2):
                row = (f // 512) * 1024 + r * 512 + f % 512
                cx.dma(kt[:, m, r * TOK:(r + 1) * TOK], T["kf_g"].t.ap()[row:row + 128, :], kt, T["kf_g"])
        for r in range(2):
            for u in range(2):
                cx.dma(vv[:, r * 16 + u:r * 16 + 16:2, :],
                       dram_ap(T["v_g"], (r * 256 + u * 128) * 2560 + 1536 + h * 256,
                               [[2560, 128], [512 * 2560, 8], [1, 256]]), vv, T["v_g"])
        cx.dma(strip[:], dram_ap(T["strip_diff"], h * 2 * 128 * DIFF_W, [[DIFF_W, 128], [128 * DIFF_W, 2], [1, DIFF_W]]),
               strip, T["strip_diff"])
    nums = [cx.ps("cnum%d" % i, [128, 512], F32) for i in range(4)]
    den = cx.ps("cden", [128, 512], F32)
    sps = [lps, cx.ps("csp1", [128, 512], F32), cx.ps("csp2", [128, 512], F32)]
    tmp = [cx.sb("ctmp%d" % i, [128, 512], F32) for i in range(2)]
    pb = [cx.sb("cpb%d" % i, [128, 512], BF16) for i in range(3)]
    rden = cx.sb("crden", [128, 8], F32)
    rl = cx.sb("crl", [128, 4], F32)
    a1 = [cx.sb("ca1%d" % i, [128, 256], F32) for i in range(2)]
    oo = [cx.sb("coo%d" % i, [128, 256], F32) for i in range(2)]
    jk = cx.sb("cjk", [128, 256], F32)
    sst = [cx.sb("csst%d" % i, [128, 4], F32) for i in range(2)]
    onn = [cx.sb("con%d" % i, [128, 256], F32) for i in range(2)]
    oT = [cx.sb("coT%d" % i, [128, 2, 512], BF16) for i in range(2)]
    n = 0
    load_head(0)
    for h in range(4):
        qt, kt, vv, strip = qt2[h % 2], kt2[h % 2], vv2[h % 2], strip2[h % 2]
        if h + 1 < 4:
            load_head(h + 1)
        for qb in range(4):
            o_T = oT[qb % 2]
            steps = [(kti, m) for kti in range(32) for m in range(2)]

            def emit_S(idx):
                kti, m = steps[idx]
                sp = sps[idx % 3]
                cx.op("pe", lambda e: e.matmul(out=sp[:], lhsT=kt[:, m, kti * 128:(kti + 1) * 128],
                                               rhs=qt[:, m, qb * 512:(qb + 1) * 512], start=True, stop=True),
                      reads=[kt, qt], writes=[sp])

            emit_S(0)
            for idx in range(64):
                kti, m = steps[idx]
                if idx + 1 < 64:
                    emit_S(idx + 1)
                sidx = kti // 16
                k0 = (kti % 16) * 128
                m0 = DIFF_OFF - (k0 - qb * 512)
                sp, tp_, p_ = sps[idx % 3], tmp[idx % 2], pb[idx % 3]
                cx.op("dve", lambda e: e.tensor_tensor(out=tp_[:], in0=sp[:], in1=strip[:, sidx, m0:m0 + 512], op=ALU.add),
                      reads=[sp, strip], writes=[tp_])
                cx.op("act", lambda e: e.activation(out=p_[:], in_=tp_[:], func=AF.Exp), reads=[tp_], writes=[p_])
                first = (idx == 0)
                last = (idx == 63)
                for qs in range(4):
                    cx.op("pe", lambda e: e.matmul(out=nums[qs][:, m * 256:(m + 1) * 256], lhsT=p_[:, qs * 128:(qs + 1) * 128],
                                                   rhs=vv[:, kti, :], start=first, stop=last, skip_group_check=True),
                          reads=[p_, vv], writes=[nums[qs]])
                    cx.op("pe", lambda e: e.matmul(out=den[:, qs * 2 + m:qs * 2 + m + 1], lhsT=p_[:, qs * 128:(qs + 1) * 128],
                                                   rhs=G["ones_bf"][:, 0:1], start=(first and qs == 0), stop=(last and qs == 3),
                                                   skip_group_check=True),
                          reads=[p_, G["ones_bf"]], writes=[den])
            cx.op("dve", lambda e: e.reciprocal(out=rden[:], in_=den[:, 0:8]), reads=[den], writes=[rden])
            cx.op("dve", lambda e: e.tensor_tensor(out=rl[:], in0=rden[:].rearrange("p (a b) -> p a b", b=2)[:, :, 1],
                                                   in1=nlam[:].broadcast_to([128, 4]), op=ALU.mult),
                  reads=[rden, nlam], writes=[rl])
            for qs in range(4):
                i = qs % 2
                cx.op("act", lambda e: e.activation(out=a1[i][:], in_=nums[qs][:, 0:256], func=AF.Copy,
                                                    scale=rden[:, qs * 2:qs * 2 + 1]), reads=[nums[qs], rden], writes=[a1[i]])
                cx.op("dve", lambda e: e.scalar_tensor_tensor(out=oo[i][:], in0=nums[qs][:, 256:512], scalar=rl[:, qs:qs + 1],
                                                              in1=a1[i][:], op0=ALU.mult, op1=ALU.add),
                      reads=[nums[qs], rl, a1[i]], writes=[oo[i]])
                cx.op("act", lambda e: e.activation(out=jk[:], in_=oo[i][:], func=AF.Square, accum_out=sst[i][:, 0:1]),
                      reads=[oo[i]], writes=[jk, sst[i]])
                cx.op("dve", lambda e: e.tensor_scalar(out=sst[i][:, 1:2], in0=sst[i][:, 0:1], scalar1=1.0 / 256, scalar2=EPS,
                                                       op0=ALU.mult, op1=ALU.add), reads=[sst[i]], writes=[sst[i]])
                cx.op("act", lambda e: e.sqrt(out=sst[i][:, 2:3], in_=sst[i][:, 1:2]), reads=[sst[i]], writes=[sst[i]])
                cx.op("dve", lambda e: e.reciprocal(out=sst[i][:, 3:4], in_=sst[i][:, 2:3]), reads=[sst[i]], writes=[sst[i]])
                cx.op("dve", lambda e: e.scalar_tensor_tensor(out=onn[i][:], in0=oo[i][:], scalar=sst[i][:, 3:4], in1=gsub[:],
                                                              op0=ALU.mult, op1=ALU.mult),
                      reads=[oo[i], sst[i], gsub], writes=[onn[i]])
                tpp = sps[n % 3]
                n += 1
                for c2 in range(2):
                    cx.op("pe", lambda e: e.transpose(out=tpp[:, c2 * 128:(c2 + 1) * 128], in_=onn[i][:, c2 * 128:(c2 + 1) * 128],
                                                      identity=G["ident_f"][:]), reads=[onn[i], G["ident_f"]], writes=[tpp])
                cx.op("act", lambda e: e.copy(out=o_T[:, :, qs * 128:(qs + 1) * 128],
                                              in_=tpp[:, 0:256].rearrange("p (a b) -> p a b", a=2)), reads=[tpp], writes=[o_T])
            cx.dma(dram_ap(T["mixT"], (1024 + h * 256) * TOK + qb * 512, [[TOK, 128], [128 * TOK, 2], [1, 512]]), o_T[:],
                   T["mixT"], o_T)
    cx.end_phase()


def phase_merge(cx, T, layer):
    cx.begin_phase()
    aT = cx.sb("maT", [128, 16, TOK], BF16)
    cx.dma(aT[:], dram_ap(T["mixT"], 0, [[TOK, 128], [128 * TOK, 16], [1, TOK]]), aT, T["mixT"])
    wup = cx.sb("mwup", [128, 16, D], BF16)
    stg = [cx.sb("mstg%d" % i, [128, 2, D], F32) for i in range(2)]
    wl = T["wl"][layer]
    srcs = [("w_up_a", wl * 512 * D, 4), ("w_up_b", wl * 512 * D, 4), ("w_up_c", wl * 1024 * D, 8)]
    ch = 0
    n = 0
    for name, base, nch in srcs:
        for c2 in range(0, nch, 2):
            s_ = stg[n % 2]
            n += 1
            cx.dma(s_[:], dram_ap(T[name], base + c2 * 128 * D, [[D, 128], [128 * D, 2], [1, D]]), s_, T[name])
            if (ch // 2) % 2 == 0:
                cx.op("act", lambda e: e.copy(out=wup[:, ch:ch + 2, :], in_=s_[:]), reads=[s_], writes=[wup])
            else:
                cx.op("dve", lambda e: e.tensor_copy(out=wup[:, ch:ch + 2, :], in_=s_[:]), reads=[s_], writes=[wup])
            ch += 2
    gt = [cx.sb("mg%d" % i, [128, 3, 512], F32) for i in range(2)]
    ps = [cx.ps("mps%d" % i, [128, 512], F32) for i in range(6)]
    t1 = [cx.sb("mt1%d" % i, [128, 512], F32) for i in range(2)]
    t2 = [cx.sb("mt2%d" % i, [128, 512], F32) for i in range(2)]
    t3 = [cx.sb("mt3%d" % i, [128, 512], F32) for i in range(2)]
    ob = [cx.sb("mob%d" % i, [128, 512], BF16) for i in range(2)]
    kr = ((0, 4), (4, 8), (8, 16))
    n = 0
    for dmt in range(16):
        for tb in range(4):
            i = n % 2
            g_ = gt[i]
            cx.dma(g_[:], dram_ap(T["gatesT"], (dmt * 128) * TOK + tb * 512, [[TOK, 128], [D * TOK, 3], [1, 512]]), g_, T["gatesT"])
            tt = (t1[i], t2[i], t3[i])
            for br in range(3):
                p = ps[i * 3 + br]
                k0, k1 = kr[br]
                for k in range(k0, k1):
                    cx.op("pe", lambda e: e.matmul(out=p[:], lhsT=wup[:, k, dmt * 128:(dmt + 1) * 128],
                                                   rhs=aT[:, k, tb * 512:(tb + 1) * 512], start=(k == k0), stop=(k == k1 - 1)),
                          reads=[wup, aT], writes=[p])
                cx.op("dve", lambda e: e.tensor_tensor(out=tt[br][:], in0=p[:], in1=g_[:, br, :], op=ALU.mult),
                      reads=[p, g_], writes=[tt[br]])
            cx.op("pool", lambda e: e.tensor_tensor(out=tt[0][:], in0=tt[0][:], in1=tt[1][:], op=ALU.add),
                  reads=[tt[0], tt[1]], writes=[tt[0]])
            cx.op("pool", lambda e: e.tensor_tensor(out=ob[i][:], in0=tt[0][:], in1=tt[2][:], op=ALU.add),
                  reads=[tt[0], tt[2]], writes=[ob[i]])
            cx.dma(T["mergedT"].t.ap()[dmt * 128:(dmt + 1) * 128, tb * 512:(tb + 1) * 512], ob[i][:], T["mergedT"], ob[i], q="pool")
            n += 1
    cx.end_phase()


def phase_wo(cx, T, layer, x_src, x_dst):
    cx.begin_phase()
    mT = cx.sb("omT", [128, 16, TOK], BF16)
    cx.dma(mT[:], dram_ap(T["mergedT"], 0, [[TOK, 128], [128 * TOK, 16], [1, TOK]]), mT, T["mergedT"])
    stg = [cx.sb("ostg%d" % i, [128, 16, 512], F32) for i in range(2)]
    wbf = [cx.sb("owbf%d" % i, [128, 16, 512], BF16) for i in range(2)]
    xs = [cx.sb("oxs%d" % i, [128, 512], F32) for i in range(3)]
    ps = [cx.ps("ops%d" % i, [128, 512], F32) for i in range(4)]
    w = T["w_o"]
    w.rowlen = D
    w.base = T["wl"][layer] * D * D
    n = 0
    load_w_bf16(cx, w, 0, 0, 512, stg[0], wbf[0], "pool")
    for cb in range(4):
        if cb + 1 < 4:
            load_w_bf16(cx, w, 0, (cb + 1) * 512, 512, stg[(cb + 1) % 2], wbf[(cb + 1) % 2], "pool")
        w_ = wbf[cb % 2]
        for tt in range(NT):
            p = ps[n % 4]
            x_ = xs[n % 3]
            n += 1
            cx.dma(x_[:], x_src.t.ap()[tt * 128:(tt + 1) * 128, cb * 512:(cb + 1) * 512], x_, x_src)
            for k in range(16):
                cx.op("pe", lambda e: e.matmul(out=p[:], lhsT=mT[:, k, tt * 128:(tt + 1) * 128], rhs=w_[:, k, :],
                                               start=(k == 0), stop=(k == 15)), reads=[mT, w_], writes=[p])
            cx.op("dve", lambda e: e.tensor_tensor(out=x_[:], in0=p[:], in1=x_[:], op=ALU.add), reads=[p, x_], writes=[x_])
            cx.dma(x_dst.t.ap()[tt * 128:(tt + 1) * 128, cb * 512:(cb + 1) * 512], x_[:], x_dst, x_, q="pool")
    cx.end_phase()


def phase_peer_q(cx, T, layer, x_src):
    cx.begin_phase()
    hT = cx.sb("phT", [128, 16, TOK], BF16)
    rmsnorm_to_hT(cx, x_src, (T["ffn_norm_g"], T["wl"][layer] * D), hT, T["G"]["ident_bf"], T)
    stg = [cx.sb("pstg%d" % i, [128, 16, 512], F32) for i in range(2)]
    wbf = [cx.sb("pwbf%d" % i, [128, 16, 512], BF16) for i in range(2)]
    ps = [cx.ps("pps%d" % i, [128, 512], F32) for i in range(4)]
    of = [cx.sb("pof%d" % i, [128, 512], F32) for i in range(3)]
    w = T["peer_wq"]
    w.rowlen = D
    w.base = T["wl"][layer] * D * D
    n = 0
    load_w_bf16(cx, w, 0, 0, 512, stg[0], wbf[0], "pool")
    for cb in range(4):
        if cb + 1 < 4:
            load_w_bf16(cx, w, 0, (cb + 1) * 512, 512, stg[(cb + 1) % 2], wbf[(cb + 1) % 2], "pool")
        w_ = wbf[cb % 2]
        for m in range(4):
            for tb in range(4):
                p = ps[n % 4]
                o = of[n % 3]
                n += 1
                for k in range(16):
                    cx.op("pe", lambda e: e.matmul(out=p[:], lhsT=w_[:, k, m * 128:(m + 1) * 128], rhs=hT[:, k, tb * 512:(tb + 1) * 512],
                                                   start=(k == 0), stop=(k == 15)), reads=[w_, hT], writes=[p])
                if n % 2 == 0:
                    cx.op("act", lambda e: e.copy(out=o[:], in_=p[:]), reads=[p], writes=[o])
                else:
                    cx.op("dve", lambda e: e.tensor_copy(out=o[:], in_=p[:]), reads=[p], writes=[o])
                row = cb * 512 + m * 128
                cx.dma(T["qT"].t.ap()[row:row + 128, tb * 512:(tb + 1) * 512], o[:], T["qT"], o, q="pool")
    cx.end_phase()


def phase_peer(cx, T, layer, x_src, x_dst):
    G = T["G"]
    cx.begin_phase()
    NEG = -1.0e30
    sk = cx.sb("sk", [128, 2, 128], F32)
    cx.dma(sk[:], dram_ap(T["peer_subkeys"], T["wl"][layer] * 2 * 128 * 128, [[128, 128], [128 * 128, 2], [1, 128]]), sk, T["peer_subkeys"])
    scp = [cx.ps("scp%d" % i, [128, 512], F32) for i in range(4)]
    acc = [cx.ps("acc%d" % i, [128, 512], F32) for i in range(4)]
    skT = cx.sb("skT", [128, 2, 128], F32)
    for p_ in range(2):
        cx.op("pe", lambda e: e.transpose(out=scp[0][:, p_ * 128:(p_ + 1) * 128], in_=sk[:, p_, :], identity=G["ident_f"][:]),
              reads=[sk, G["ident_f"]], writes=[scp[0]])
    cx.op("act", lambda e: e.copy(out=skT[:].rearrange("p a b -> p (a b)"), in_=scp[0][:, 0:256]), reads=[scp[0]], writes=[skT])
    ids_l = [cx.sb("ids%d" % i, [128, 128], I32) for i in range(NT)]
    gat_l = [cx.sb("gat%d" % i, [128, 128], F32) for i in range(NT)]
    qTt = [cx.sb("qTt%d" % i, [128, 16, 128], F32) for i in range(2)]
    sc = cx.sb("sc", [128, 16, 128], F32)
    scr = cx.sb("scr", [128, 256], F32)
    m8 = cx.sb("m8", [128, 16, 16], F32)
    i8 = cx.sb("i8", [128, 16, 16], U32)
    i8f = cx.sb("i8f", [128, 16, 16], F32)
    cand = cx.sb("cand", [128, 8, 256], F32)
    bs = cx.sb("bs", [128, 8, 16], F32)
    bj = cx.sb("bj", [128, 8, 16], U32)
    ja = cx.sb("ja", [128, 8, 16], U32)
    jb = cx.sb("jb", [128, 8, 16], U32)
    jaf = cx.sb("jaf", [128, 8, 16], F32)
    jbf = cx.sb("jbf", [128, 8, 16], F32)
    eq = cx.sb("eq", [128, 8, 16, 16], F32)
    sel1 = cx.sb("sel1", [128, 8, 16], F32)
    sel2 = cx.sb("sel2", [128, 8, 16], F32)
    ex = cx.sb("ex", [128, 8, 16], F32)
    esum = cx.sb("esum", [128, 8], F32)
    m8v = m8[:].rearrange("p (h two) k -> p h two k", two=2)
    i8v = i8f[:].rearrange("p (h two) k -> p h two k", two=2)
    B4 = [128, 8, 16, 16]
    def topk_gen(tt):
            q_ = qTt[tt % 2]
            cx.dma(q_[:], dram_ap(T["qT"], tt * 128, [[TOK, 128], [128 * TOK, 16], [1, 128]]), q_, T["qT"])
            yield
            for hp in range(16):
                cx.op("pe", lambda e: e.matmul(out=scp[hp // 4][:, (hp % 4) * 128:(hp % 4 + 1) * 128], lhsT=q_[:, hp, :],
                                               rhs=skT[:, hp % 2, :], start=True, stop=True, skip_group_check=True),
                      reads=[q_, skT], writes=[scp[hp // 4]])
                yield
            for b4 in range(4):
                cx.op("act", lambda e: e.copy(out=sc[:, b4 * 4:(b4 + 1) * 4, :].rearrange("p a b -> p (a b)"), in_=scp[b4][:]),
                      reads=[scp[b4]], writes=[sc])
                yield
            for hp in range(16):
                cx.op("dve", lambda e: e.max(out=m8[:, hp, 0:8], in_=sc[:, hp, :]), reads=[sc], writes=[m8])
                yield
                cx.op("dve", lambda e: e.match_replace(out=scr[:, 0:128], in_to_replace=m8[:, hp, 0:8], in_values=sc[:, hp, :],
                                                       imm_value=NEG), reads=[m8, sc], writes=[scr])
                yield
                cx.op("dve", lambda e: e.max(out=m8[:, hp, 8:16], in_=scr[:, 0:128]), reads=[scr], writes=[m8])
                yield
                cx.op("dve", lambda e: e.max_index(out=i8[:, hp, 0:8], in_max=m8[:, hp, 0:8], in_values=sc[:, hp, :]),
                      reads=[m8, sc], writes=[i8])
                yield
                cx.op("dve", lambda e: e.max_index(out=i8[:, hp, 8:16], in_max=m8[:, hp, 8:16], in_values=sc[:, hp, :]),
                      reads=[m8, sc], writes=[i8])
                yield
            cx.op("dve", lambda e: e.tensor_copy(out=i8f[:], in_=i8[:]), reads=[i8], writes=[i8f])
            yield
            cx.op("dve", lambda e: e.tensor_tensor(out=cand[:].rearrange("p h (a b) -> p h a b", b=16),
                                                   in0=m8v[:, :, 0, :].unsqueeze(3).broadcast_to(B4),
                                                   in1=m8v[:, :, 1, :].unsqueeze(2).to_broadcast(B4), op=ALU.add),
                  reads=[m8], writes=[cand])
            yield
            for h in range(8):
                cx.op("dve", lambda e: e.max(out=bs[:, h, 0:8], in_=cand[:, h, :]), reads=[cand], writes=[bs])
                yield
                cx.op("dve", lambda e: e.match_replace(out=scr[:], in_to_replace=bs[:, h, 0:8], in_values=cand[:, h, :],
                                                       imm_value=NEG), reads=[bs, cand], writes=[scr])
                yield
                cx.op("dve", lambda e: e.max(out=bs[:, h, 8:16], in_=scr[:]), reads=[scr], writes=[bs])
                yield
                cx.op("dve", lambda e: e.max_index(out=bj[:, h, 0:8], in_max=bs[:, h, 0:8], in_values=cand[:, h, :]),
                      reads=[bs, cand], writes=[bj])
                yield
                cx.op("dve", lambda e: e.max_index(out=bj[:, h, 8:16], in_max=bs[:, h, 8:16], in_values=cand[:, h, :]),
                      reads=[bs, cand], writes=[bj])
                yield
            cx.op("dve", lambda e: e.tensor_single_scalar(out=ja[:], in_=bj[:], scalar=4, op=ALU.logical_shift_right),
                  reads=[bj], writes=[ja])
            yield
            cx.op("dve", lambda e: e.tensor_single_scalar(out=jb[:], in_=bj[:], scalar=15, op=ALU.bitwise_and),
                  reads=[bj], writes=[jb])
            yield
            cx.op("dve", lambda e: e.tensor_copy(out=jaf[:], in_=ja[:]), reads=[ja], writes=[jaf])
            yield
            cx.op("dve", lambda e: e.tensor_copy(out=jbf[:], in_=jb[:]), reads=[jb], writes=[jbf])
            yield
            iob = G["iota16"][:].unsqueeze(1).unsqueeze(1).to_broadcast(B4)
            for (jf, pp, sel) in ((jaf, 0, sel1), (jbf, 1, sel2)):
                cx.op("dve", lambda e: e.tensor_tensor(out=eq[:], in0=jf[:].unsqueeze(3).broadcast_to(B4), in1=iob, op=ALU.is_equal),
                      reads=[jf, G["iota16"]], writes=[eq])
                yield
                cx.op("dve", lambda e: e.tensor_tensor(out=eq[:], in0=eq[:], in1=i8v[:, :, pp, :].unsqueeze(2).to_broadcast(B4),
                                                       op=ALU.mult), reads=[eq, i8f], writes=[eq])
                yield
                cx.op("dve", lambda e: e.tensor_reduce(out=sel[:], in_=eq[:], axis=AX.X, op=ALU.add), reads=[eq], writes=[sel])
                yield
            cx.op("dve", lambda e: e.tensor_scalar(out=sel1[:], in0=sel1[:], scalar1=128.0, scalar2=float(T["wl"][layer] * 16384),
                                                   op0=ALU.mult, op1=ALU.add), reads=[sel1], writes=[sel1])
            yield
            cx.op("dve", lambda e: e.tensor_tensor(out=sel1[:], in0=sel1[:], in1=sel2[:], op=ALU.add), reads=[sel1, sel2], writes=[sel1])
            yield
            cx.op("dve", lambda e: e.tensor_copy(out=ids_l[tt][:], in_=sel1[:].rearrange("p h k -> p (h k)")),
                  reads=[sel1], writes=[ids_l[tt]])
            yield
            cx.op("dve", lambda e: e.tensor_tensor(out=ex[:], in0=bs[:], in1=bs[:, :, 0:1].broadcast_to([128, 8, 16]), op=ALU.subtract),
                  reads=[bs], writes=[ex])
            yield
            cx.op("act", lambda e: e.activation(out=ex[:], in_=ex[:], func=AF.Exp), reads=[ex], writes=[ex])
            yield
            cx.op("dve", lambda e: e.tensor_reduce(out=esum[:], in_=ex[:], axis=AX.X, op=ALU.add), reads=[ex], writes=[esum])
            yield
            cx.op("dve", lambda e: e.reciprocal(out=esum[:], in_=esum[:]), reads=[esum], writes=[esum])
            yield
            cx.op("dve", lambda e: e.tensor_tensor(out=gat_l[tt][:].rearrange("p (h k) -> p h k", k=16), in0=ex[:],
                                                   in1=esum[:].unsqueeze(2).broadcast_to([128, 8, 16]), op=ALU.mult),
                  reads=[ex, esum], writes=[gat_l[tt]])
            yield
    NB = 6
    uvb = [cx.sb("uvb%d" % i, [128, 2 * D], BF16) for i in range(NB)]
    xt = [cx.sb("pxt%d" % i, [128, D], F32) for i in range(2)]
    hf = [cx.sb("phf%d" % i, [128, D], BF16) for i in range(2)]
    gb = cx.sb("pgb", [128, D], F32)
    cx.dma(gb[:], dram_ap(T["ffn_norm_g"], T["wl"][layer] * D, [[0, 128], [1, D]]), gb, T["ffn_norm_g"])
    junkp = cx.sb("pjunkp", [128, D], BF16)
    junk2 = [cx.sb("pjunk%d" % i, [128, D], BF16) for i in range(2)]
    st = [cx.sb("pst%d" % i, [128, 4], F32) for i in range(2)]
    pre_s = [cx.sb("ppre%d" % i, [128, 1], F32) for i in range(4)]
    gl_s = [cx.sb("pgl%d" % i, [128, 1], F32) for i in range(4)]
    dg = [cx.sb("pdg%d" % i, [128, 128], BF16) for i in range(4)]
    uvtab = T["uv16"].t.ap()
    idm = G["ident_bf"]

    def prep(tt):
        i = tt % 2
        x_t, s_t, h_f = xt[i], st[i], hf[i]
        cx.dma(x_t[:], x_src.t.ap()[tt * 128:(tt + 1) * 128, :], x_t, x_src)
        cx.op("act", lambda e: e.activation(out=junkp[:], in_=x_t[:], func=AF.Square, accum_out=s_t[:, 0:1]),
              reads=[x_t], writes=[junkp, s_t])
        cx.op("dve", lambda e: e.tensor_scalar(out=s_t[:, 1:2], in0=s_t[:, 0:1], scalar1=1.0 / D, scalar2=EPS,
                                               op0=ALU.mult, op1=ALU.add), reads=[s_t], writes=[s_t])
        cx.op("act", lambda e: e.sqrt(out=s_t[:, 2:3], in_=s_t[:, 1:2]), reads=[s_t], writes=[s_t])
        cx.op("dve", lambda e: e.reciprocal(out=s_t[:, 3:4], in_=s_t[:, 2:3]), reads=[s_t], writes=[s_t])
        cx.op("dve", lambda e: e.scalar_tensor_tensor(out=h_f[:], in0=x_t[:], scalar=s_t[:, 3:4], in1=gb[:],
                                                      op0=ALU.mult, op1=ALU.mult), reads=[x_t, s_t, gb], writes=[h_f])

    def v_step(tt, j, uv_):
        d_ = dg[j % 4]
        gl = gl_s[j % 4]
        cx.op("dve", lambda e: e.tensor_scalar(out=d_[:], in0=idm[:], scalar1=gl[:, 0:1], scalar2=gat_l[tt][:, j:j + 1],
                                               op0=ALU.mult, op1=ALU.mult), reads=[idm, gl, gat_l[tt]], writes=[d_])
        for c in range(4):
            cx.op("pe", lambda e: e.matmul(out=acc[c][:], lhsT=d_[:], rhs=uv_[:, D + c * 512:D + (c + 1) * 512],
                                           start=(j == 0), stop=(j == 127)), reads=[d_, uv_], writes=[acc[c]])

    def fin(tt):
        x_t = xt[tt % 2]
        for c in range(4):
            cx.op("dve", lambda e: e.tensor_tensor(out=x_t[:, c * 512:(c + 1) * 512], in0=acc[c][:], in1=x_t[:, c * 512:(c + 1) * 512],
                                                   op=ALU.add), reads=[acc[c], x_t], writes=[x_t])
        cx.dma(x_dst.t.ap()[tt * 128:(tt + 1) * 128, :], x_t[:], x_dst, x_t)

    ng = 0
    for _ in topk_gen(0):
        pass
    prep(0)
    for tt in range(NT):
        i = tt % 2
        prev = None
        gen = topk_gen(tt + 1) if tt + 1 < NT else iter(())
        for j in range(128):
            next(gen, None)
            next(gen, None)
            uv_ = uvb[ng % NB]
            jk = junk2[ng % 2]
            ng += 1
            cx.dma(None, None, uv_, T["uv16"], q="pool",
                   fn=lambda e: e.indirect_dma_start(out=uv_[:], out_offset=None, in_=uvtab,
                                                     in_offset=bass.IndirectOffsetOnAxis(ap=ids_l[tt][:, j:j + 1], axis=0)),
                   extra_reads=[ids_l[tt]])
            pr = pre_s[j % 4]
            cx.op("dve", lambda e: e.scalar_tensor_tensor(out=jk[:], in0=uv_[:, 0:D], scalar=1.0, in1=hf[i][:], op0=ALU.mult,
                                                          op1=ALU.mult, accum_out=pr[:, 0:1]), reads=[uv_, hf[i]], writes=[jk, pr])
            if prev is not None:
                v_step(tt, prev[0], prev[1])
            gl = gl_s[j % 4]
            cx.op("act", lambda e: e.activation(out=gl[:], in_=pr[:], func=AF.Gelu), reads=[pr], writes=[gl])
            prev = (j, uv_)
            if j == 64 and tt + 1 < NT:
                prep(tt + 1)
        v_step(tt, prev[0], prev[1])
        for _ in gen:
            pass
        fin(tt)
    cx.end_phase()


def phase_final(cx, T, x_src, out_dst):
    cx.begin_phase()
    gb = cx.sb("fgb", [128, D], F32)
    cx.dma(gb[:], dram_ap(T["final_norm_g"], 0, [[0, 128], [1, D]]), gb, T["final_norm_g"])
    xt = [cx.sb("fxt%d" % i, [128, D], F32) for i in range(2)]
    yo = [cx.sb("fyo%d" % i, [128, D], F32) for i in range(2)]
    junk = cx.sb("fjunk", [128, D], BF16)
    st = [cx.sb("fst%d" % i, [128, 4], F32) for i in range(2)]
    for tt in range(NT):
        i = tt % 2
        x_t, s_t = xt[i], st[i]
        cx.dma(x_t[:], x_src.t.ap()[tt * 128:(tt + 1) * 128, :], x_t, x_src)
        cx.op("act", lambda e: e.activation(out=junk[:], in_=x_t[:], func=AF.Square, accum_out=s_t[:, 0:1]),
              reads=[x_t], writes=[junk, s_t])
        cx.op("dve", lambda e: e.tensor_scalar(out=s_t[:, 1:2], in0=s_t[:, 0:1], scalar1=1.0 / D, scalar2=EPS,
                                               op0=ALU.mult, op1=ALU.add), reads=[s_t], writes=[s_t])
        cx.op("act", lambda e: e.sqrt(out=s_t[:, 2:3], in_=s_t[:, 1:2]), reads=[s_t], writes=[s_t])
        cx.op("dve", lambda e: e.reciprocal(out=s_t[:, 3:4], in_=s_t[:, 2:3]), reads=[s_t], writes=[s_t])
        cx.op("dve", lambda e: e.scalar_tensor_tensor(out=yo[i][:], in0=x_t[:], scalar=s_t[:, 3:4], in1=gb[:],
                                                      op0=ALU.mult, op1=ALU.mult), reads=[x_t, s_t, gb], writes=[yo[i]])
        cx.dma(out_dst.t.ap()[tt * 128:(tt + 1) * 128, :], yo[i][:], out_dst, yo[i])
    cx.end_phase()


import ml_dtypes

BF = ml_dtypes.bfloat16


def rel_bucket_np(rel):
    rel = np.asarray(rel, dtype=np.int64)
    half, max_exact = 16, 8
    n = np.abs(rel)
    nf = np.maximum(n, 1).astype(np.float32) / np.float32(max_exact)
    big = max_exact + (np.log(nf) / np.float32(math.log(2048 / max_exact)) * np.float32(half - max_exact)).astype(np.int32)
    big = np.minimum(big, half - 1)
    return np.where(rel > 0, half, 0) + np.where(n < max_exact, n, big)


_CONST_CACHE = {}


def host_consts(half):
    if half in _CONST_CACHE:
        return _CONST_CACHE[half]
    c = {}
    c["c_ident_bf"] = np.eye(128, dtype=np.float32).astype(BF)
    c["c_ident_f"] = np.eye(128, dtype=np.float32)
    c["c_J"] = np.ascontiguousarray(np.eye(128, dtype=np.float32)[::-1])
    c["c_iota16"] = np.tile(np.arange(16, dtype=np.float32)[None, :], (128, 1))
    cc = np.arange(128)
    th = 2.0 * np.pi * ((cc[:, None] * cc[None, :]) % 128) / 128.0
    c["c_dftc"] = np.concatenate([np.cos(th), -np.sin(th)], axis=1).astype(np.float32).astype(BF)
    s_ = np.arange(SEQ, dtype=np.int64)
    k_ = half * TOK + np.arange(TOK, dtype=np.int64)
    ph = ((s_[:, None] * k_[None, :]) % SEQ).astype(np.float64) * (2.0 * np.pi / SEQ)
    c["c_dfts"] = np.stack([np.cos(ph), np.sin(ph)], axis=0).astype(np.float32).astype(BF)
    idx = np.arange(4096)
    oh = np.zeros((32, 2, 4096), np.float32)
    for s2, sh in ((0, (0 - half) * 2048), (1, (1 - half) * 2048)):
        rel = 2047 - idx + sh
        b = rel_bucket_np(rel)
        valid = idx < 4095
        oh[b[valid], s2, idx[valid]] = 1.0
    c["c_oh_diff"] = oh
    ohl = np.zeros((33, 3, 512), np.float32)
    for g, (d, rad) in enumerate(DIL):
        for t in range(2):
            for ix in range(255):
                nn = 127 - ix
                if t == 0:
                    ok = nn >= 0
                    rel = (nn - 64) * d
                else:
                    ok = nn <= 0
                    rel = (nn + 64) * d
                if ok:
                    ohl[int(rel_bucket_np(rel)), g, t * 256 + ix] = 1.0
                else:
                    ohl[32, g, t * 256 + ix] = 1.0
    c["c_oh_dil"] = ohl
    hm = np.zeros((128, 2), np.float32)
    hm[:, half] = 1.0
    c["c_half"] = hm
    _CONST_CACHE[half] = c
    return c


CONST_SPECS = [("c_ident_bf", [128, 128], BF16), ("c_ident_f", [128, 128], F32), ("c_J", [128, 128], F32),
               ("c_iota16", [128, 16], F32), ("c_dftc", [128, 256], BF16), ("c_dfts", [2, SEQ, TOK], BF16),
               ("c_oh_diff", [32, 2, 4096], F32), ("c_oh_dil", [33, 3, 512], F32), ("c_half", [128, 2], F32)]

WEIGHT_SPECS = [("mix_norm_g", [D]), ("w_in", [D, INW]), ("b_gate", [6144]), ("w_up_a", [512, D]), ("w_up_b", [512, D]),
                ("w_up_c", [1024, D]), ("diff_lambda", [4, 128]), ("diff_subln_g", [256]), ("w_o", [D, D]),
                ("ffn_norm_g", [D]), ("peer_wq", [D, D]), ("peer_subkeys", [2, 128, 128]), ("peer_u", [16384, D]),
                ("peer_v", [16384, D])]

A_OUT = [("kfT", [3072, TOK], BF16), ("qdT", [1536, TOK], BF16), ("qcT", [1024, TOK], BF16), ("vtok", [TOK, 2560], BF16),
         ("gatesT", [6144, TOK], F32)]
EXCH = [("fz", [512, SEQ], BF16), ("kd_ext", [1536, 3 * TOK], BF16), ("vd_ext", [3 * TOK, 12, 129], BF16),
        ("kc_oo", [1024, SEQ], BF16), ("vc_oo", [SEQ, 1024], BF16)]


def phase_exchange(cx, T):
    G = T["G"]
    if not T.get("ag_done"):
        for c in range(6):
            cx.allgather(T["kfT"], T["kfT"].t.ap()[c * 512:(c + 1) * 512, :], T["kf_g"], T["kf_g"].t.ap()[c * 1024:(c + 1) * 1024, :])
        for c in range(8):
            cx.allgather(T["vtok"], T["vtok"].t.ap()[c * 256:(c + 1) * 256, :], T["v_g"], T["v_g"].t.ap()[c * 512:(c + 1) * 512, :])
    T["ag_done"] = False
    cx.begin_phase()
    src = [cx.sb("xv%d" % i, [128, 1536], BF16) for i in range(3)]
    dst = [cx.sb("xo%d" % i, [128, 12, 129], BF16) for i in range(3)]
    ones = cx.sb("xones", [128, 12], F32)
    cx.op("dve", lambda e: e.memset(ones[:], 1.0), writes=[ones])
    n = 0
    for region in range(3):
        for tile in range(NT):
            s_, d_ = src[n % 3], dst[n % 3]
            n += 1
            if region == 1:
                cx.dma(s_[:], T["vtok"].t.ap()[tile * 128:(tile + 1) * 128, 0:1536], s_, T["vtok"])
                cx.op("dve", lambda e: e.tensor_copy(out=d_[:, :, 0:128], in_=s_[:].rearrange("p (h e) -> p h e", e=128)),
                      reads=[s_], writes=[d_])
                cx.op("dve", lambda e: e.tensor_copy(out=d_[:, :, 128], in_=ones[:]), reads=[ones, d_], writes=[d_])
            else:
                r = 0 if region == 0 else 1
                msk = G["half"][:, 1:2] if region == 0 else G["half"][:, 0:1]
                row = (tile // 2) * 512 + r * 256 + (tile % 2) * 128
                cx.dma(s_[:], T["v_g"].t.ap()[row:row + 128, 0:1536], s_, T["v_g"])
                cx.op("dve", lambda e: e.tensor_scalar(out=d_[:, :, 0:128], in0=s_[:].rearrange("p (h e) -> p h e", e=128),
                                                       scalar1=msk, scalar2=None, op0=ALU.mult),
                      reads=[s_, G["half"]], writes=[d_])
                cx.op("dve", lambda e: e.tensor_scalar(out=d_[:, :, 128], in0=ones[:], scalar1=msk, scalar2=None, op0=ALU.mult),
                      reads=[ones, G["half"], d_], writes=[d_])
            r0 = region * TOK + tile * 128
            cx.dma(T["vd_ext"].t.ap()[r0:r0 + 128, :, :], d_[:], T["vd_ext"], d_)
    cx.end_phase()


def build_fused():
    nc = bass.Bass("TRN2", target_bir_lowering=False)
    cx = Ctx(nc)
    T = {}

    def ext_in(name, shape, dt):
        T[name] = cx.dram(name, shape, dt, "ExternalInput")

    def scratch(name, shape, dt):
        T[name] = cx.dram(name, shape, dt, "ExternalOutput" if (DEBUG_OUT and name in DEBUG_OUT) else "Internal")

    for name, shape, dt in CONST_SPECS:
        ext_in(name, shape, dt)
    ext_in("x_in", [TOK, D], F32)
    ext_in("rel_bias", [32, 16], F32)
    ext_in("final_norm_g", [D], F32)
    for name, shape in WEIGHT_SPECS:
        ext_in(name, [2] + shape, F32)
    T["out"] = cx.dram("out", [TOK, D], F32, "ExternalOutput")
    for name, shape, dt in A_OUT:
        scratch(name, shape, dt)
    for l in range(2):
        scratch("kf_g%d" % l, [6 * 2 * 512, TOK], BF16)
        scratch("v_g%d" % l, [8 * 2 * 256, 2560], BF16)
    scratch("vd_ext", [3 * TOK, 12, 129], BF16)
    scratch("rb_diff", [4, 2, 4096], F32)
    scratch("rows_dil", [3, 12, 512], F32)
    scratch("strip_diff", [4, 2, 128, DIFF_W], F32)
    scratch("tm_dil", [12, 128, 256], F32)
    scratch("numd", [3, TOK, 4, 129], F32)
    scratch("mixT", [2048, TOK], BF16)
    scratch("mergedT", [D, TOK], BF16)
    scratch("qT", [D, TOK], F32)
    scratch("xb", [TOK, D], F32)
    scratch("xc", [TOK, D], F32)
    if PEER_BF16:
        scratch("uv16", [2 * 16384, 2 * D], BF16)
    T["wl"] = {0: 0, 1: 1}
    T["G"] = load_consts(cx, T)
    phase_bias(cx, T)
    x_cur = T["x_in"]
    for layer in range(NLAYERS):
        T["kf_g"] = T["kf_g%d" % layer]
        T["v_g"] = T["v_g%d" % layer]
        phase_A(cx, layer, x_cur, T)
        phase_exchange(cx, T)
        phase_fnet(cx, T)
        phase_dil(cx, T)
        phase_diff(cx, T, layer)
        phase_merge(cx, T, layer)
        phase_wo(cx, T, layer, x_cur, T["xb"])
        phase_peer_q(cx, T, layer, T["xb"])
        phase_peer(cx, T, layer, T["xb"], T["xc"])
        x_cur = T["xc"]
    phase_final(cx, T, x_cur, T["out"])
    cx.finish()
    return nc


NLAYERS = 2
PEER_BF16 = True
_PROG = {}


def kernel(**inputs):
    inp = {k: np.asarray(v) for k, v in inputs.items()}
    x = inp["x"].astype(np.float32, copy=False)
    cores = list(range(8))
    if "p" not in _PROG:
        _PROG["p"] = build_fused()
    maps = []
    for c in cores:
        m = dict(host_consts(c % 2))
        m["rel_bias"] = inp["rel_bias"]
        m["final_norm_g"] = inp["final_norm_g"]
        for name, shape in WEIGHT_SPECS:
            m[name] = inp[name]
        m["x_in"] = np.ascontiguousarray(x[c // 2, (c % 2) * TOK:(c % 2 + 1) * TOK])
        maps.append(m)
    res = run_bass_kernel_spmd(_PROG["p"], maps, core_ids=cores).results
    out = np.empty((4, SEQ, D), np.float32)
    for c in cores:
        out[c // 2, (c % 2) * TOK:(c % 2 + 1) * TOK] = res[c]["out"]
    return out
```

```python
import math
from contextlib import ExitStack

import numpy as np
import concourse.bass as bass
import concourse.mybir as mybir
from concourse.bass_utils import run_bass_kernel_spmd

F32 = mybir.dt.float32
BF16 = mybir.dt.bfloat16
I32 = mybir.dt.int32
U32 = mybir.dt.uint32
AF = mybir.ActivationFunctionType
ALU = mybir.AluOpType
AX = mybir.AxisListType

D = 2048
SEQ = 4096
TOK = 2048
NT = TOK // 128
INW = 14336
EPS = 1e-6
SAME_SYNC = True
DEBUG_OUT = False
DEBUG_STOP = None


class Sem:
    def __init__(self, h):
        self.h = h
        self.cnt = 0


class Buf:
    def __init__(self, name, t, space):
        self.name = name
        self.t = t
        self.space = space
        self.w = {}
        self.r = {}
        self.sem = None

    def __getitem__(self, idx):
        return self.t[idx]

    def ap(self):
        return self.t.ap() if self.space == "dram" else self.t[:]


class Ctx:
    def __init__(self, nc):
        self.nc = nc
        self.gs = ExitStack()
        self.eng = {"pe": nc.tensor, "act": nc.scalar, "dve": nc.vector,
                    "pool": nc.gpsimd, "sp": nc.sync}
        self.esem = {}
        for k in self.eng:
            self.esem[k] = Sem(self.gs.enter_context(nc.semaphore("es_" + k)))
        self.waited = {k: {} for k in self.eng}
        self.free_sems = [Sem(self.gs.enter_context(nc.semaphore("ds_%d" % i))) for i in range(72)]
        self.all_sems = list(self.free_sems)
        self.phase_stack = None
        self.phase_bufs = []
        self.dram_bufs = []
        self.uid = 0

    def begin_phase(self):
        self.scopes = [(ExitStack(), [])]
        self.phase_stack, self.phase_bufs = self.scopes[-1]

    def push_scope(self):
        self.scopes.append((ExitStack(), []))
        self.phase_stack, self.phase_bufs = self.scopes[-1]

    def pop_scope(self):
        self.barrier()
        st, bufs = self.scopes.pop()
        for b in bufs:
            if b.sem is not None:
                self.free_sems.append(b.sem)
                b.sem = None
        st.close()
        if self.scopes:
            self.phase_stack, self.phase_bufs = self.scopes[-1]
        else:
            self.phase_stack, self.phase_bufs = None, []

    def end_phase(self):
        while self.scopes:
            self.pop_scope()

    def sb(self, name, shape, dtype):
        self.uid += 1
        t = self.phase_stack.enter_context(self.nc.sbuf_tensor("%s_%d" % (name, self.uid), list(shape), dtype))
        b = Buf(name, t, "sb")
        self.phase_bufs.append(b)
        return b

    def ps(self, name, shape, dtype=F32):
        self.uid += 1
        t = self.phase_stack.enter_context(self.nc.psum_tensor("%s_%d" % (name, self.uid), list(shape), dtype))
        b = Buf(name, t, "ps")
        self.phase_bufs.append(b)
        return b

    def sbg(self, name, shape, dtype):
        self.uid += 1
        t = self.gs.enter_context(self.nc.sbuf_tensor("%s_%d" % (name, self.uid), list(shape), dtype))
        return Buf(name, t, "sb")

    def dram(self, name, shape, dtype, kind="Internal"):
        if kind == "Internal":
            t = self.nc.dram_tensor(name, list(shape), dtype)
        else:
            t = self.nc.dram_tensor(name, list(shape), dtype, kind=kind)
        b = Buf(name, t, "dram")
        self.dram_bufs.append(b)
        return b

    def _need(self, e, events):
        eng = self.eng[e]
        wd = self.waited[e]
        for ev in events:
            sem, val = ev
            if sem is self.esem.get(e):
                if e == "pe" or not SAME_SYNC:
                    continue
            if wd.get(id(sem), 0) >= val:
                continue
            eng.wait_ge(sem.h, val)
            wd[id(sem)] = val

    @staticmethod
    def _resolve(evs):
        out = []
        for ev in evs:
            if ev[0] == "dma":
                out.append((ev[1], ev[1].cnt))
            else:
                out.append(ev)
        return out

    def op(self, e, fn, reads=(), writes=(), lax=None):
        evs = []
        lax_ids = set(id(b) for b in lax) if lax else ()
        own = self.esem[e]
        for b in reads:
            evs.extend(b.w.values())
        for b in writes:
            wr = list(b.w.values()) + list(b.r.values())
            if id(b) in lax_ids:
                wr = [ev for ev in wr if ev[0] is not own]
            evs.extend(wr)
        evs = self._resolve(evs)
        self._need(e, evs)
        inst = fn(self.eng[e])
        s = self.esem[e]
        s.cnt += 1
        inst.then_inc(s.h, 1)
        me = (s, s.cnt)
        for b in reads:
            b.r[e] = me
        for b in writes:
            b.w = {e: me}
            b.r = {}
        return inst

    def dma(self, out_ap, in_ap, dst, src, q="sp", fn=None, slow=False, extra_reads=()):
        owner = dst if dst.space == "sb" else (src if src.space == "sb" else dst)
        if owner.sem is None:
            owner.sem = self.free_sems.pop()
        evs = list(src.w.values())
        for b in extra_reads:
            evs.extend(b.w.values())
        if dst.space != "dram":
            evs.extend(dst.w.values())
            evs.extend(dst.r.values())
        self._need(q, self._resolve(evs))
        if fn is None:
            if slow:
                inst = self.eng[q].dma_start(out=out_ap, in_=in_ap, allow_slow_non_contiguous=True)
            else:
                inst = self.eng[q].dma_start(out=out_ap, in_=in_ap)
        else:
            inst = fn(self.eng[q])
        owner.sem.cnt += 16
        inst.then_inc(owner.sem.h, 16)
        ev = ("dma", owner.sem)
        key = id(owner.sem)
        src.r[key] = ev
        for b in extra_reads:
            b.r[key] = ev
        if dst.space == "dram":
            dst.w[key] = ev
        else:
            dst.w = {key: ev}
            dst.r = {}
        return inst

    def allgather(self, src, src_ap, dst, dst_ap):
        evs = list(src.w.values()) + list(dst.w.values()) + list(dst.r.values())
        self._need("pool", self._resolve(evs))
        if dst.sem is None:
            dst.sem = self.free_sems.pop()
        inst = self.eng["pool"].collective_compute("AllGather", ALU.bypass, replica_groups=[[0, 1], [2, 3], [4, 5], [6, 7]],
                                                   ins=[src_ap.opt()], outs=[dst_ap.opt()])
        dst.sem.cnt += 1
        inst.then_inc(dst.sem.h)
        ev = (dst.sem, dst.sem.cnt)
        dst.w[id(dst.sem)] = ev
        src.r[id(dst.sem)] = ev
        return inst

    def barrier(self):
        evs = [(s, s.cnt) for s in self.esem.values()]
        evs += [(s, s.cnt) for s in self.all_sems if s.cnt > 0]
        save = SAME_SYNC
        for e in self.eng:
            wd = self.waited[e]
            for sem, val in evs:
                if wd.get(id(sem), 0) >= val:
                    continue
                self.eng[e].wait_ge(sem.h, val)
                wd[id(sem)] = val
        for b in self.dram_bufs:
            b.w = {}
            b.r = {}

    def finish(self):
        self.barrier()
        self.gs.close()


def dram_ap(buf, offset, pattern):
    return bass.AP(tensor=buf.t, offset=offset, ap=[list(p) for p in pattern])


def load_consts(cx, T):
    G = {}
    G["ident_bf"] = cx.sbg("identbf", [128, 128], BF16)
    cx.dma(G["ident_bf"][:], T["c_ident_bf"].t.ap(), G["ident_bf"], T["c_ident_bf"])
    G["ident_f"] = cx.sbg("identf", [128, 128], F32)
    cx.dma(G["ident_f"][:], T["c_ident_f"].t.ap(), G["ident_f"], T["c_ident_f"])
    G["J"] = cx.sbg("Jf", [128, 128], F32)
    cx.dma(G["J"][:], T["c_J"].t.ap(), G["J"], T["c_J"])
    G["iota16"] = cx.sbg("iota16", [128, 16], F32)
    cx.dma(G["iota16"][:], T["c_iota16"].t.ap(), G["iota16"], T["c_iota16"])
    if "c_half" in T:
        G["half"] = cx.sbg("halfm", [128, 2], F32)
        cx.dma(G["half"][:], T["c_half"].t.ap(), G["half"], T["c_half"])
    G["ones_bf"] = cx.sbg("onesbf", [128, 2], BF16)
    cx.op("dve", lambda e: e.memset(G["ones_bf"][:], 1.0), writes=[G["ones_bf"]])
    return G


def rmsnorm_to_hT(cx, x_dram, g_dram_row, hT, ident_bf, consts, xf_keep=None):
    nc = cx.nc
    cx.push_scope()
    gb = cx.sb("gb", [128, D], F32)
    gbuf, goff = g_dram_row
    cx.dma(gb[:], dram_ap(gbuf, goff, [[0, 128], [1, D]]), gb, gbuf)
    xt = [cx.sb("xt%d" % i, [128, D], F32) for i in range(2)]
    junk = cx.sb("junk", [128, D], BF16)
    xs = [cx.sb("xs%d" % i, [128, D], BF16) for i in range(2)]
    st = [cx.sb("st%d" % i, [128, 4], F32) for i in range(2)]
    tp = [cx.ps("tp%d" % i, [128, 8, 128], BF16) for i in range(2)]
    for tt in range(NT):
        x_t = xt[tt % 2]
        s_t = st[tt % 2]
        x_s = xs[tt % 2]
        cx.dma(x_t[:], x_dram.t.ap()[tt * 128:(tt + 1) * 128, :], x_t, x_dram)
        cx.op("act", lambda e: e.activation(out=junk[:], in_=x_t[:], func=AF.Square, accum_out=s_t[:, 0:1]),
              reads=[x_t], writes=[junk, s_t])
        cx.op("dve", lambda e: e.tensor_scalar(out=s_t[:, 1:2], in0=s_t[:, 0:1], scalar1=1.0 / D, scalar2=EPS,
                                               op0=ALU.mult, op1=ALU.add), reads=[s_t], writes=[s_t])
        cx.op("act", lambda e: e.sqrt(out=s_t[:, 2:3], in_=s_t[:, 1:2]), reads=[s_t], writes=[s_t])
        cx.op("dve", lambda e: e.reciprocal(out=s_t[:, 3:4], in_=s_t[:, 2:3]), reads=[s_t], writes=[s_t])
        cx.op("dve", lambda e: e.scalar_tensor_tensor(out=x_s[:], in0=x_t[:], scalar=s_t[:, 3:4], in1=gb[:],
                                                      op0=ALU.mult, op1=ALU.mult), reads=[x_t, s_t, gb], writes=[x_s])
        for half in range(2):
            p = tp[half]
            for j in range(8):
                k = half * 8 + j
                cx.op("pe", lambda e: e.transpose(out=p[:, j, :], in_=x_s[:, k * 128:(k + 1) * 128], identity=ident_bf[:]),
                      reads=[x_s, ident_bf], writes=[p])
            eng = "act" if half == 0 else "dve"
            if eng == "act":
                cx.op("act", lambda e: e.copy(out=hT[:, half * 8:(half + 1) * 8, tt * 128:(tt + 1) * 128], in_=p[:]),
                      reads=[p], writes=[hT])
            else:
                cx.op("dve", lambda e: e.tensor_copy(out=hT[:, half * 8:(half + 1) * 8, tt * 128:(tt + 1) * 128], in_=p[:]),
                      reads=[p], writes=[hT])
    cx.pop_scope()


def load_w_bf16(cx, wbuf, row0, col0, ncols, stg, wbf, conv_eng):
    rowlen = wbuf.rowlen
    src = dram_ap(wbuf, wbuf.base + row0 * rowlen + col0, [[rowlen, 128], [128 * rowlen, 16], [1, ncols]])
    cx.dma(stg[:, :, 0:ncols], src, stg, wbuf)
    if conv_eng == "pool":
        cx.op("act", lambda e: e.copy(out=wbf[:, 0:8, 0:ncols], in_=stg[:, 0:8, 0:ncols]), reads=[stg], writes=[wbf])
        cx.op("dve", lambda e: e.tensor_copy(out=wbf[:, 8:16, 0:ncols], in_=stg[:, 8:16, 0:ncols]), reads=[stg], writes=[wbf])
    elif conv_eng == "dve":
        cx.op("dve", lambda e: e.tensor_copy(out=wbf[:, :, 0:ncols], in_=stg[:, :, 0:ncols]), reads=[stg], writes=[wbf])
    else:
        cx.op("act", lambda e: e.copy(out=wbf[:, :, 0:ncols], in_=stg[:, :, 0:ncols]), reads=[stg], writes=[wbf])


AG_AFTER = {0: [("k", 0)], 4: [("k", 1)], 5: [("k", 2)], 6: [("k", 3)], 12: [("k", 4)], 13: [("k", 5)],
            15: [("v", c) for c in range(8)]}


def conv_begin(cx, T, layer):
    T["conv"] = {"layer": layer, "next": 0, "total": 256,
                 "cin": [cx.sb("cvi%d" % i, [128, D], F32) for i in range(3)],
                 "cout": [cx.sb("cvo%d" % i, [128, D], BF16) for i in range(3)]}


def conv_step(cx, T, ntiles):
    st = T.get("conv")
    if st is None:
        return
    wl = T["wl"][st["layer"]]

    def finish_tile(t):
        dstn = "uv16"
        row0 = wl * 16384 + (t % 128) * 128
        ci, co = st["cin"][t % 3], st["cout"][t % 3]
        if t % 2 == 0:
            cx.op("act", lambda e: e.copy(out=co[:], in_=ci[:]), reads=[ci], writes=[co])
        else:
            cx.op("dve", lambda e: e.tensor_copy(out=co[:], in_=ci[:]), reads=[ci], writes=[co])
        cx.dma(dram_ap(T[dstn], row0 * 2 * D + (0 if t < 128 else D), [[2 * D, 128], [1, D]]), co[:], T[dstn], co, q="pool")

    for _ in range(ntiles):
        t = st["next"]
        if t > st["total"]:
            return
        st["next"] = t + 1
        if t < st["total"]:
            tab = "peer_u" if t < 128 else "peer_v"
            row0 = wl * 16384 + (t % 128) * 128
            ci = st["cin"][t % 3]
            cx.dma(ci[:], dram_ap(T[tab], row0 * D, [[D, 128], [1, D]]), ci, T[tab])
        if t >= 1:
            finish_tile(t - 1)


def phase_A(cx, layer, x_dram, T):
    cx.begin_phase()
    ident_bf = T["G"]["ident_bf"]
    hT = cx.sb("hT", [128, 16, TOK], BF16)
    rmsnorm_to_hT(cx, x_dram, (T["mix_norm_g"], T["wl"][layer] * D), hT, ident_bf, T)
    bg = cx.sb("bgate", [128, 48], F32)
    cx.dma(bg[:], dram_ap(T["b_gate"], T["wl"][layer] * 6144, [[1, 128], [128, 48]]), bg, T["b_gate"], slow=True)
    stg = [cx.sb("wstg%d" % i, [128, 16, 512], F32) for i in range(2)]
    wbf = [cx.sb("wbf%d" % i, [128, 16, 512], BF16) for i in range(2)]
    if "uv16" in T:
        conv_begin(cx, T, layer)
    pss = [cx.ps("psA%d" % i, [128, 512], F32) for i in range(4)]
    obf = [cx.sb("obf%d" % i, [128, 512], BF16) for i in range(3)]
    of32 = [cx.sb("of32%d" % i, [128, 512], F32) for i in range(3)]
    w_in = T["w_in"]
    w_in.rowlen = INW
    w_in.base = T["wl"][layer] * D * INW
    cnt = {"ps": 0, "o": 0, "ev": 0}
    qscale = 128 ** -0.5
    NCB = INW // 512

    def w_dma(cb):
        src = dram_ap(w_in, w_in.base + cb * 512, [[INW, 128], [128 * INW, 16], [1, 512]])
        cx.dma(stg[cb % 2][:], src, stg[cb % 2], w_in)

    def w_cast(cb):
        s2, w2 = stg[cb % 2], wbf[cb % 2]
        cx.op("act", lambda e: e.copy(out=w2[:, 0:8, :], in_=s2[:, 0:8, :]), reads=[s2], writes=[w2])
        cx.op("dve", lambda e: e.tensor_copy(out=w2[:, 8:16, :], in_=s2[:, 8:16, :]), reads=[s2], writes=[w2])

    w_dma(0)
    w_cast(0)
    for cb in range(NCB):
        c0 = cb * 512
        s_, w_ = stg[cb % 2], wbf[cb % 2]
        if cb + 1 < NCB:
            w_dma(cb + 1)
        if c0 < 512:
            kind, dest, r0, scale = "f", T["kfT"], c0, 1.0
        elif c0 < 2048:
            kind, dest, r0, scale = "f", T["qdT"], c0 - 512, qscale
        elif c0 < 3584:
            kind, dest, r0, scale = "f", T["kfT"], 512 + (c0 - 2048), 1.0
        elif c0 < 5120:
            kind, dest, r0, scale = "v", T["vtok"], c0 - 3584, 1.0
        elif c0 < 6144:
            kind, dest, r0, scale = "f", T["qcT"], c0 - 5120, qscale
        elif c0 < 7168:
            kind, dest, r0, scale = "f", T["kfT"], 2048 + (c0 - 6144), 1.0
        elif c0 < 8192:
            kind, dest, r0, scale = "v", T["vtok"], 1536 + (c0 - 7168), 1.0
        else:
            kind, dest, r0, scale = "g", T["gatesT"], c0 - 8192, 1.0
        step = 0
        if kind in ("f", "g"):
            for m in range(4):
                for tb in range(4):
                    if step < 10:
                        conv_step(cx, T, 1)
                    if step == 8 and cb + 1 < NCB:
                        w_cast(cb + 1)
                    step += 1
                    p = pss[cnt["ps"] % 4]
                    cnt["ps"] += 1
                    for k in range(16):
                        cx.op("pe", lambda e: e.matmul(out=p[:], lhsT=w_[:, k, m * 128:(m + 1) * 128],
                                                       rhs=hT[:, k, tb * 512:(tb + 1) * 512],
                                                       start=(k == 0), stop=(k == 15)),
                              reads=[w_, hT], writes=[p])
                    row = r0 + m * 128
                    if kind == "f":
                        o = obf[cnt["o"] % 3]
                        cnt["o"] += 1
                        if cnt["ev"] % 2 == 0:
                            cx.op("act", lambda e: e.activation(out=o[:], in_=p[:], func=AF.Copy, scale=scale),
                                  reads=[p], writes=[o])
                        else:
                            cx.op("dve", lambda e: e.tensor_scalar(out=o[:], in0=p[:], scalar1=scale, scalar2=None,
                                                                   op0=ALU.mult), reads=[p], writes=[o])
                        sq = "act" if cnt["ev"] % 2 == 0 else "pool"
                        cnt["ev"] += 1
                        cx.dma(dest.t.ap()[row:row + 128, tb * 512:(tb + 1) * 512], o[:], dest, o, q=sq)
                    else:
                        o = of32[cnt["o"] % 3]
                        cnt["o"] += 1
                        gt = (c0 - 8192) // 128 + m
                        cx.op("act", lambda e: e.activation(out=o[:], in_=p[:], func=AF.Sigmoid,
                                                            bias=bg[:, gt:gt + 1], scale=1.0),
                              reads=[p, bg], writes=[o])
                        cx.dma(dest.t.ap()[row:row + 128, tb * 512:(tb + 1) * 512], o[:], dest, o, q="act")
        else:
            for tt in range(NT):
                if step < 10:
                    conv_step(cx, T, 1)
                if step == 8 and cb + 1 < NCB:
                    w_cast(cb + 1)
                step += 1
                p = pss[cnt["ps"] % 4]
                cnt["ps"] += 1
                for k in range(16):
                    cx.op("pe", lambda e: e.matmul(out=p[:], lhsT=hT[:, k, tt * 128:(tt + 1) * 128],
                                                   rhs=w_[:, k, :], start=(k == 0), stop=(k == 15)),
                          reads=[w_, hT], writes=[p])
                o = obf[cnt["o"] % 3]
                cnt["o"] += 1
                if cnt["ev"] % 2 == 0:
                    cx.op("act", lambda e: e.copy(out=o[:], in_=p[:]), reads=[p], writes=[o])
                else:
                    cx.op("dve", lambda e: e.tensor_copy(out=o[:], in_=p[:]), reads=[p], writes=[o])
                sq = "act" if cnt["ev"] % 2 == 0 else "pool"
                cnt["ev"] += 1
                cx.dma(dest.t.ap()[tt * 128:(tt + 1) * 128, r0:r0 + 512], o[:], dest, o, q=sq)
        if "kf_g" in T:
            for kind_, c in AG_AFTER.get(cb, ()):
                if kind_ == "k":
                    cx.allgather(T["kfT"], T["kfT"].t.ap()[c * 512:(c + 1) * 512, :], T["kf_g"],
                                 T["kf_g"].t.ap()[c * 1024:(c + 1) * 1024, :])
                else:
                    cx.allgather(T["vtok"], T["vtok"].t.ap()[c * 256:(c + 1) * 256, :], T["v_g"],
                                 T["v_g"].t.ap()[c * 512:(c + 1) * 512, :])
            T["ag_done"] = True
    conv_step(cx, T, 256)
    T["conv"] = None
    cx.end_phase()


DIFF_OFF = 1920
DIFF_W = 3968
DIL = ((1, 64), (4, 64), (16, 64))


def phase_bias(cx, T):
    G = T["G"]
    cx.begin_phase()
    rb = cx.sb("rb33", [33, 16], F32)
    cx.op("dve", lambda e: e.memset(rb[:], -30000.0), writes=[rb])
    cx.dma(rb[0:32, :], T["rel_bias"].t.ap(), rb, T["rel_bias"])
    ohd = cx.sb("ohd", [32, 2, 4096], F32)
    cx.dma(ohd[:], T["c_oh_diff"].t.ap(), ohd, T["c_oh_diff"])
    ohl = cx.sb("ohl", [33, 3, 512], F32)
    cx.dma(ohl[:], T["c_oh_dil"].t.ap(), ohl, T["c_oh_dil"])
    ps = [cx.ps("pb%d" % i, [128, 512], F32) for i in range(2)]
    rows = cx.sb("rows", [16, 512], F32)
    n = 0
    for s_ in range(2):
        for c in range(8):
            p = ps[n % 2]
            n += 1
            cx.op("pe", lambda e: e.matmul(out=p[0:4, :], lhsT=rb[0:32, 12:16], rhs=ohd[:, s_, c * 512:(c + 1) * 512],
                                           start=True, stop=True), reads=[rb, ohd], writes=[p])
            cx.op("act", lambda e: e.copy(out=rows[0:4, :], in_=p[0:4, :]), reads=[p], writes=[rows])
            cx.dma(dram_ap(T["rb_diff"], s_ * 4096 + c * 512, [[8192, 4], [1, 512]]), rows[0:4, :], T["rb_diff"], rows)
    for g in range(3):
        p = ps[n % 2]
        n += 1
        cx.op("pe", lambda e: e.matmul(out=p[0:12, :], lhsT=rb[0:33, 0:12], rhs=ohl[:, g, :], start=True, stop=True),
              reads=[rb, ohl], writes=[p])
        cx.op("act", lambda e: e.copy(out=rows[0:12, :], in_=p[0:12, :]), reads=[p], writes=[rows])
        cx.dma(dram_ap(T["rows_dil"], g * 12 * 512, [[512, 12], [1, 512]]), rows[0:12, :], T["rows_dil"], rows)
    hk = [cx.sb("hk%d" % i, [128, DIFF_W], F32) for i in range(2)]
    ob = [cx.sb("ob%d" % i, [128, 512], F32) for i in range(2)]
    m = 0
    for h in range(4):
        for s_ in range(2):
            hb = hk[m % 2]
            m += 1
            cx.dma(hb[:], dram_ap(T["rb_diff"], h * 8192 + s_ * 4096, [[1, 128], [1, DIFF_W]]), hb, T["rb_diff"])
            for c in range(0, DIFF_W, 512):
                w = min(512, DIFF_W - c)
                p = ps[n % 2]
                o = ob[n % 2]
                n += 1
                cx.op("pe", lambda e: e.matmul(out=p[:, 0:w], lhsT=G["J"][:], rhs=hb[:, c:c + w], start=True, stop=True),
                      reads=[G["J"], hb], writes=[p])
                cx.op("act", lambda e: e.copy(out=o[:, 0:w], in_=p[:, 0:w]), reads=[p], writes=[o])
                cx.dma(dram_ap(T["strip_diff"], ((h * 2 + s_) * 128) * DIFF_W + c, [[DIFF_W, 128], [1, w]]), o[:, 0:w],
                       T["strip_diff"], o)
    hk2 = [cx.sb("hk2%d" % i, [128, 2, 128], F32) for i in range(2)]
    for g in range(3):
        for hh in range(4):
            hd = g * 4 + hh
            hb = hk2[m % 2]
            m += 1
            cx.dma(hb[:], dram_ap(T["rows_dil"], (g * 12 + hd) * 512, [[1, 128], [256, 2], [1, 128]]), hb, T["rows_dil"])
            p = ps[n % 2]
            o = ob[n % 2]
            n += 1
            cx.op("pe", lambda e: e.matmul(out=p[:, 0:256], lhsT=G["J"][:], rhs=hb[:].rearrange("p a b -> p (a b)"),
                                           start=True, stop=True), reads=[G["J"], hb], writes=[p])
            cx.op("act", lambda e: e.copy(out=o[:, 0:256], in_=p[:, 0:256]), reads=[p], writes=[o])
            cx.dma(dram_ap(T["tm_dil"], hd * 128 * 256, [[256, 128], [1, 256]]), o[:, 0:256], T["tm_dil"], o)
    cx.end_phase()


def phase_fnet(cx, T):
    cx.begin_phase()
    zT = cx.sb("zT", [128, 4, SEQ], BF16)
    if "kf_g" in T:
        for r in range(2):
            cx.dma(zT[:, :, r * TOK:(r + 1) * TOK], dram_ap(T["kf_g"], (r * 512) * TOK, [[TOK, 128], [128 * TOK, 4], [1, TOK]]),
                   zT, T["kf_g"])
    else:
        cx.dma(zT[:], dram_ap(T["fz"], 0, [[SEQ, 128], [128 * SEQ, 4], [1, SEQ]]), zT, T["fz"])
    dc = cx.sb("dftc", [128, 256], BF16)
    cx.dma(dc[:], T["c_dftc"].t.ap(), dc, T["c_dftc"])
    zz = [cx.sb("zz%d" % g, [128, 32, 256], BF16) for g in range(4)]
    ps = [cx.ps("pf%d" % i, [128, 512], F32) for i in range(4)]
    n = 0
    for g in range(4):
        for s2 in range(16):
            p = ps[n % 4]
            for u in range(2):
                st = s2 * 2 + u
                cx.op("pe", lambda e: e.matmul(out=p[:, u * 256:(u + 1) * 256], lhsT=zT[:, g, st * 128:(st + 1) * 128],
                                               rhs=dc[:], start=True, stop=True), reads=[zT, dc], writes=[p])
            if n % 2 == 0:
                cx.op("act", lambda e: e.copy(out=zz[g][:, s2 * 2:s2 * 2 + 2, :].rearrange("p a b -> p (a b)"), in_=p[:]),
                      reads=[p], writes=[zz[g]])
            else:
                cx.op("dve", lambda e: e.tensor_copy(out=zz[g][:, s2 * 2:s2 * 2 + 2, :].rearrange("p a b -> p (a b)"), in_=p[:]),
                      reads=[p], writes=[zz[g]])
            n += 1
    cs = cx.sb("csb", [128, 2, 32, 512], BF16)
    ob = [cx.sb("fo%d" % i, [128, 512], BF16) for i in range(2)]
    sc = 1.0 / math.sqrt(SEQ * 128.0)
    for kb in range(4):
        for t in range(2):
            cx.dma(cs[:, t, :, :], dram_ap(T["c_dfts"], t * SEQ * TOK + kb * 512, [[TOK, 128], [128 * TOK, 32], [1, 512]]),
                   cs, T["c_dfts"])
        for g in range(4):
            p = ps[n % 4]
            o = ob[n % 2]
            n += 1
            i = 0
            for st in range(32):
                for t in range(2):
                    cx.op("pe", lambda e: e.matmul(out=p[:], lhsT=zz[g][:, st, t * 128:(t + 1) * 128], rhs=cs[:, t, st, :],
                                                   start=(i == 0), stop=(i == 63)), reads=[zz[g], cs], writes=[p])
                    i += 1
            cx.op("act", lambda e: e.activation(out=o[:], in_=p[:], func=AF.Copy, scale=sc), reads=[p], writes=[o])
            cx.dma(T["mixT"].t.ap()[g * 128:(g + 1) * 128, kb * 512:(kb + 1) * 512], o[:], T["mixT"], o)
    cx.end_phase()


def phase_dil(cx, T):
    G = T["G"]
    cx.begin_phase()
    RS = 12 * 129
    qt = [cx.sb("dq%d" % i, [128, TOK], BF16) for i in range(2)]
    kt = [cx.sb("dk%d" % i, [128, 3 * TOK], BF16) for i in range(2)]
    tm = [cx.sb("dtm%d" % i, [128, 2, 128], F32) for i in range(2)]
    vt = [cx.sb("dv%d" % i, [128, 2, 129], BF16) for i in range(4)]
    sps = [cx.ps("dsp%d" % i, [128, 2, 128], F32) for i in range(3)]
    ops = [cx.ps("dop%d" % i, [128, 129], F32) for i in range(3)]
    tmp = [cx.sb("dtmp%d" % i, [128, 2, 128], F32) for i in range(3)]
    pb = [cx.sb("dpb%d" % i, [128, 2, 128], BF16) for i in range(3)]
    osb = [cx.sb("dosb%d" % i, [128, 129], F32) for i in range(3)]
    n = 0
    for g in range(3):
        d = DIL[g][0]
        nloc = TOK // d
        for hh in range(4):
            hd = g * 4 + hh
            q_, k_, tm_ = qt[hd % 2], kt[hd % 2], tm[hd % 2]
            cx.dma(q_[:], T["qdT"].t.ap()[hd * 128:(hd + 1) * 128, :], q_, T["qdT"])
            if "kf_g" in T:
                f = 512 + hd * 128
                cx.dma(k_[:, TOK:2 * TOK], T["kfT"].t.ap()[f:f + 128, :], k_, T["kfT"])
                for r in range(2):
                    row = (f // 512) * 1024 + r * 512 + f % 512
                    dst = k_[:, 0:TOK] if r == 0 else k_[:, 2 * TOK:3 * TOK]
                    cx.dma(dst, T["kf_g"].t.ap()[row:row + 128, :], k_, T["kf_g"])
                    msk = G["half"][:, 1:2] if r == 0 else G["half"][:, 0:1]
                    cx.op("dve", lambda e: e.tensor_scalar(out=dst, in0=dst, scalar1=msk, scalar2=None, op0=ALU.mult),
                          reads=[k_, G["half"]], writes=[k_])
            else:
                cx.dma(k_[:], T["kd_ext"].t.ap()[hd * 128:(hd + 1) * 128, :], k_, T["kd_ext"])
            cx.dma(tm_[:], dram_ap(T["tm_dil"], hd * 128 * 256, [[256, 128], [128, 2], [1, 128]]), tm_, T["tm_dil"])
            blocks = [(r, blk) for r in range(d) for blk in range(nloc // 128)]

            def geom(bi):
                r, blk = blocks[bi]
                m0 = blk * 128
                row0 = TOK + (m0 - 64) * d + r
                qs = m0 * d + r
                return row0, qs

            def emit_S(bi, nn):
                row0, qs = geom(bi)
                sp = sps[nn % 3]
                qap = q_[:, qs:qs + 127 * d + 1:d] if d > 1 else q_[:, qs:qs + 128]
                for t in range(2):
                    ks = row0 + t * 128 * d
                    kap = k_[:, ks:ks + 127 * d + 1:d] if d > 1 else k_[:, ks:ks + 128]
                    cx.op("pe", lambda e: e.matmul(out=sp[:, t, :], lhsT=kap, rhs=qap, start=True, stop=True),
                          reads=[k_, q_], writes=[sp])

            emit_S(0, n)
            for bi in range(len(blocks)):
                row0, qs = geom(bi)
                v_ = vt[n % 4]
                sp, op_, tp_, p_, o_ = sps[n % 3], ops[n % 3], tmp[n % 3], pb[n % 3], osb[n % 3]
                cx.dma(v_[:], dram_ap(T["vd_ext"], row0 * RS + hd * 129, [[d * RS, 128], [128 * d * RS, 2], [1, 129]]),
                       v_, T["vd_ext"])
                cx.op("dve", lambda e: e.tensor_tensor(out=tp_[:], in0=sp[:], in1=tm_[:], op=ALU.add),
                      reads=[sp, tm_], writes=[tp_])
                cx.op("act", lambda e: e.activation(out=p_[:], in_=tp_[:], func=AF.Exp), reads=[tp_], writes=[p_])
                if bi + 1 < len(blocks):
                    emit_S(bi + 1, n + 1)
                for t in range(2):
                    cx.op("pe", lambda e: e.matmul(out=op_[:], lhsT=p_[:, t, :], rhs=v_[:, t, :], start=(t == 0), stop=(t == 1)),
                          reads=[p_, v_], writes=[op_])
                cx.op("act", lambda e: e.copy(out=o_[:], in_=op_[:]), reads=[op_], writes=[o_])
                cx.dma(dram_ap(T["numd"], g * TOK * 4 * 129 + (qs * 4 + hh) * 129, [[d * 4 * 129, 128], [1, 129]]), o_[:],
                       T["numd"], o_, q="pool")
                n += 1
    nb = [cx.sb("dnb%d" % i, [128, 3, 4 * 129], F32) for i in range(2)]
    sm = [cx.sb("dsm%d" % i, [128, 4, 129], F32) for i in range(2)]
    rd = [cx.sb("drd%d" % i, [128, 4], F32) for i in range(2)]
    on = [cx.sb("don%d" % i, [128, 4, 128], F32) for i in range(2)]
    tps = [cx.ps("dtp%d" % i, [128, 4, 128], F32) for i in range(2)]
    oT = [cx.sb("doT%d" % i, [128, 4, 128], BF16) for i in range(2)]
    for tt in range(NT):
        i = tt % 2
        cx.dma(nb[i][:], dram_ap(T["numd"], tt * 128 * 516, [[516, 128], [TOK * 516, 3], [1, 516]]), nb[i], T["numd"])
        smf = sm[i][:].rearrange("p a b -> p (a b)")
        cx.op("dve", lambda e: e.tensor_tensor(out=smf, in0=nb[i][:, 0, :], in1=nb[i][:, 1, :], op=ALU.add),
              reads=[nb[i]], writes=[sm[i]])
        cx.op("dve", lambda e: e.tensor_tensor(out=smf, in0=smf, in1=nb[i][:, 2, :], op=ALU.add),
              reads=[nb[i], sm[i]], writes=[sm[i]])
        cx.op("dve", lambda e: e.reciprocal(out=rd[i][:], in_=sm[i][:, :, 128]), reads=[sm[i]], writes=[rd[i]])
        cx.op("dve", lambda e: e.tensor_tensor(out=on[i][:], in0=sm[i][:, :, 0:128],
                                               in1=rd[i][:].unsqueeze(2).broadcast_to([128, 4, 128]), op=ALU.mult),
              reads=[sm[i], rd[i]], writes=[on[i]])
        for hh in range(4):
            cx.op("pe", lambda e: e.transpose(out=tps[i][:, hh, :], in_=on[i][:, hh, :], identity=G["ident_f"][:]),
                  reads=[on[i], G["ident_f"]], writes=[tps[i]])
        cx.op("act", lambda e: e.copy(out=oT[i][:], in_=tps[i][:]), reads=[tps[i]], writes=[oT[i]])
        cx.dma(dram_ap(T["mixT"], 512 * TOK + tt * 128, [[TOK, 128], [128 * TOK, 4], [1, 128]]), oT[i][:], T["mixT"], oT[i])
    cx.end_phase()


def phase_diff(cx, T, layer):
    G = T["G"]
    lambda_init = 0.8 - 0.6 * math.exp(-0.3 * layer)
    cx.begin_phase()
    lam = cx.sb("lam", [1, 512], F32)
    cx.dma(lam[:], dram_ap(T["diff_lambda"], T["wl"][layer] * 512, [[512, 1], [1, 512]]), lam, T["diff_lambda"])
    lj = cx.sb("lamj", [1, 128], F32)
    ls = cx.sb("lams", [1, 4], F32)
    for i in range(2):
        cx.op("dve", lambda e: e.scalar_tensor_tensor(out=lj[:], in0=lam[:, i * 256:i * 256 + 128], scalar=1.0,
                                                      in1=lam[:, i * 256 + 128:i * 256 + 256], op0=ALU.mult, op1=ALU.mult,
                                                      accum_out=ls[:, i:i + 1]), reads=[lam], writes=[lj, ls])
    cx.op("act", lambda e: e.activation(out=ls[:, 2:4], in_=ls[:, 0:2], func=AF.Exp), reads=[ls], writes=[ls])
    cx.op("dve", lambda e: e.tensor_tensor(out=ls[:, 0:1], in0=ls[:, 2:3], in1=ls[:, 3:4], op=ALU.subtract),
          reads=[ls], writes=[ls])
    cx.op("dve", lambda e: e.tensor_scalar(out=ls[:, 1:2], in0=ls[:, 0:1], scalar1=lambda_init, scalar2=-1.0,
                                           op0=ALU.add, op1=ALU.mult), reads=[ls], writes=[ls])
    ones1 = cx.sb("ones1", [1, 128], F32)
    cx.op("dve", lambda e: e.memset(ones1[:], 1.0), writes=[ones1])
    lps = cx.ps("lps", [128, 512], F32)
    cx.op("pe", lambda e: e.matmul(out=lps[:, 0:1], lhsT=ones1[:], rhs=ls[:, 1:2], start=True, stop=True),
          reads=[ones1, ls], writes=[lps])
    nlam = cx.sb("nlam", [128, 1], F32)
    cx.op("act", lambda e: e.copy(out=nlam[:], in_=lps[:, 0:1]), reads=[lps], writes=[nlam])
    gsub = cx.sb("gsub", [128, 256], F32)
    cx.dma(gsub[:], dram_ap(T["diff_subln_g"], T["wl"][layer] * 256, [[0, 128], [1, 256]]), gsub, T["diff_subln_g"])
    cx.op("act", lambda e: e.mul(out=gsub[:], in_=gsub[:], mul=1.0 - lambda_init), reads=[gsub], writes=[gsub])
    qt2 = [cx.sb("cq%d" % i, [128, 2, TOK], BF16) for i in range(2)]
    kt2 = [cx.sb("ck%d" % i, [128, 2, SEQ], BF16) for i in range(2)]
    vv2 = [cx.sb("cv%d" % i, [128, 32, 256], BF16) for i in range(2)]
    strip2 = [cx.sb("cstrip%d" % i, [128, 2, DIFF_W], F32) for i in range(2)]

    def load_head(h):
        qt, kt, vv, strip = qt2[h % 2], kt2[h % 2], vv2[h % 2], strip2[h % 2]
        cx.dma(qt[:], dram_ap(T["qcT"], h * 256 * TOK, [[TOK, 128], [128 * TOK, 2], [1, TOK]]), qt, T["qcT"])
        for m in range(2):
            f = 2048 + h * 256 + m * 128
            for r in range(2):
                row = (f // 512) * 1024 + r * 512 + f % 512
                cx.dma(kt[:, m, r * TOK:(r + 1) * TOK], T["kf_g"].t.ap()[row:row + 128, :], kt, T["kf_g"])
        for r in range(2):
            for u in range(2):
                cx.dma(vv[:, r * 16 + u:r * 16 + 16:2, :],
                       dram_ap(T["v_g"], (r * 256 + u * 128) * 2560 + 1536 + h * 256,
                               [[2560, 128], [512 * 2560, 8], [1, 256]]), vv, T["v_g"])
        cx.dma(strip[:], dram_ap(T["strip_diff"], h * 2 * 128 * DIFF_W, [[DIFF_W, 128], [128 * DIFF_W, 2], [1, DIFF_W]]),
               strip, T["strip_diff"])
    nums = [cx.ps("cnum%d" % i, [128, 512], F32) for i in range(4)]
    den = cx.ps("cden", [128, 512], F32)
    sps = [lps, cx.ps("csp1", [128, 512], F32), cx.ps("csp2", [128, 512], F32)]
    tmp = [cx.sb("ctmp%d" % i, [128, 512], F32) for i in range(2)]
    pb = [cx.sb("cpb%d" % i, [128, 512], BF16) for i in range(3)]
    rden = cx.sb("crden", [128, 8], F32)
    rl = cx.sb("crl", [128, 4], F32)
    a1 = [cx.sb("ca1%d" % i, [128, 256], F32) for i in range(2)]
    oo = [cx.sb("coo%d" % i, [128, 256], F32) for i in range(2)]
    jk = cx.sb("cjk", [128, 256], F32)
    sst = [cx.sb("csst%d" % i, [128, 4], F32) for i in range(2)]
    onn = [cx.sb("con%d" % i, [128, 256], F32) for i in range(2)]
    oT = [cx.sb("coT%d" % i, [128, 2, 512], BF16) for i in range(2)]
    n = 0
    load_head(0)
    for h in range(4):
        qt, kt, vv, strip = qt2[h % 2], kt2[h % 2], vv2[h % 2], strip2[h % 2]
        if h + 1 < 4:
            load_head(h + 1)
        for qb in range(4):
            o_T = oT[qb % 2]
            steps = [(kti, m) for kti in range(32) for m in range(2)]

            def emit_S(idx):
                kti, m = steps[idx]
                sp = sps[idx % 3]
                cx.op("pe", lambda e: e.matmul(out=sp[:], lhsT=kt[:, m, kti * 128:(kti + 1) * 128],
                                               rhs=qt[:, m, qb * 512:(qb + 1) * 512], start=True, stop=True),
                      reads=[kt, qt], writes=[sp])

            emit_S(0)
            for idx in range(64):
                kti, m = steps[idx]
                if idx + 1 < 64:
                    emit_S(idx + 1)
                sidx = kti // 16
                k0 = (kti % 16) * 128
                m0 = DIFF_OFF - (k0 - qb * 512)
                sp, tp_, p_ = sps[idx % 3], tmp[idx % 2], pb[idx % 3]
                cx.op("dve", lambda e: e.tensor_tensor(out=tp_[:], in0=sp[:], in1=strip[:, sidx, m0:m0 + 512], op=ALU.add),
                      reads=[sp, strip], writes=[tp_])
                cx.op("act", lambda e: e.activation(out=p_[:], in_=tp_[:], func=AF.Exp), reads=[tp_], writes=[p_])
                first = (idx == 0)
                last = (idx == 63)
                for qs in range(4):
                    cx.op("pe", lambda e: e.matmul(out=nums[qs][:, m * 256:(m + 1) * 256], lhsT=p_[:, qs * 128:(qs + 1) * 128],
                                                   rhs=vv[:, kti, :], start=first, stop=last, skip_group_check=True),
                          reads=[p_, vv], writes=[nums[qs]])
                    cx.op("pe", lambda e: e.matmul(out=den[:, qs * 2 + m:qs * 2 + m + 1], lhsT=p_[:, qs * 128:(qs + 1) * 128],
                                                   rhs=G["ones_bf"][:, 0:1], start=(first and qs == 0), stop=(last and qs == 3),
                                                   skip_group_check=True),
                          reads=[p_, G["ones_bf"]], writes=[den])
            cx.op("dve", lambda e: e.reciprocal(out=rden[:], in_=den[:, 0:8]), reads=[den], writes=[rden])
            cx.op("dve", lambda e: e.tensor_tensor(out=rl[:], in0=rden[:].rearrange("p (a b) -> p a b", b=2)[:, :, 1],
                                                   in1=nlam[:].broadcast_to([128, 4]), op=ALU.mult),
                  reads=[rden, nlam], writes=[rl])
            for qs in range(4):
                i = qs % 2
                cx.op("act", lambda e: e.activation(out=a1[i][:], in_=nums[qs][:, 0:256], func=AF.Copy,
                                                    scale=rden[:, qs * 2:qs * 2 + 1]), reads=[nums[qs], rden], writes=[a1[i]])
                cx.op("dve", lambda e: e.scalar_tensor_tensor(out=oo[i][:], in0=nums[qs][:, 256:512], scalar=rl[:, qs:qs + 1],
                                                              in1=a1[i][:], op0=ALU.mult, op1=ALU.add),
                      reads=[nums[qs], rl, a1[i]], writes=[oo[i]])
                cx.op("act", lambda e: e.activation(out=jk[:], in_=oo[i][:], func=AF.Square, accum_out=sst[i][:, 0:1]),
                      reads=[oo[i]], writes=[jk, sst[i]])
                cx.op("dve", lambda e: e.tensor_scalar(out=sst[i][:, 1:2], in0=sst[i][:, 0:1], scalar1=1.0 / 256, scalar2=EPS,
                                                       op0=ALU.mult, op1=ALU.add), reads=[sst[i]], writes=[sst[i]])
                cx.op("act", lambda e: e.sqrt(out=sst[i][:, 2:3], in_=sst[i][:, 1:2]), reads=[sst[i]], writes=[sst[i]])
                cx.op("dve", lambda e: e.reciprocal(out=sst[i][:, 3:4], in_=sst[i][:, 2:3]), reads=[sst[i]], writes=[sst[i]])
                cx.op("dve", lambda e: e.scalar_tensor_tensor(out=onn[i][:], in0=oo[i][:], scalar=sst[i][:, 3:4], in1=gsub[:],
                                                              op0=ALU.mult, op1=ALU.mult),
                      reads=[oo[i], sst[i], gsub], writes=[onn[i]])
                tpp = sps[n % 3]
                n += 1
                for c2 in range(2):
                    cx.op("pe", lambda e: e.transpose(out=tpp[:, c2 * 128:(c2 + 1) * 128], in_=onn[i][:, c2 * 128:(c2 + 1) * 128],
                                                      identity=G["ident_f"][:]), reads=[onn[i], G["ident_f"]], writes=[tpp])
                cx.op("act", lambda e: e.copy(out=o_T[:, :, qs * 128:(qs + 1) * 128],
                                              in_=tpp[:, 0:256].rearrange("p (a b) -> p a b", a=2)), reads=[tpp], writes=[o_T])
            cx.dma(dram_ap(T["mixT"], (1024 + h * 256) * TOK + qb * 512, [[TOK, 128], [128 * TOK, 2], [1, 512]]), o_T[:],
                   T["mixT"], o_T)
    cx.end_phase()


def phase_merge(cx, T, layer):
    cx.begin_phase()
    aT = cx.sb("maT", [128, 16, TOK], BF16)
    cx.dma(aT[:], dram_ap(T["mixT"], 0, [[TOK, 128], [128 * TOK, 16], [1, TOK]]), aT, T["mixT"])
    wup = cx.sb("mwup", [128, 16, D], BF16)
    stg = [cx.sb("mstg%d" % i, [128, 2, D], F32) for i in range(2)]
    wl = T["wl"][layer]
    srcs = [("w_up_a", wl * 512 * D, 4), ("w_up_b", wl * 512 * D, 4), ("w_up_c", wl * 1024 * D, 8)]
    ch = 0
    n = 0
    for name, base, nch in srcs:
        for c2 in range(0, nch, 2):
            s_ = stg[n % 2]
            n += 1
            cx.dma(s_[:], dram_ap(T[name], base + c2 * 128 * D, [[D, 128], [128 * D, 2], [1, D]]), s_, T[name])
            if (ch // 2) % 2 == 0:
                cx.op("act", lambda e: e.copy(out=wup[:, ch:ch + 2, :], in_=s_[:]), reads=[s_], writes=[wup])
            else:
                cx.op("dve", lambda e: e.tensor_copy(out=wup[:, ch:ch + 2, :], in_=s_[:]), reads=[s_], writes=[wup])
            ch += 2
    gt = [cx.sb("mg%d" % i, [128, 3, 512], F32) for i in range(2)]
    ps = [cx.ps("mps%d" % i, [128, 512], F32) for i in range(6)]
    t1 = [cx.sb("mt1%d" % i, [128, 512], F32) for i in range(2)]
    t2 = [cx.sb("mt2%d" % i, [128, 512], F32) for i in range(2)]
    t3 = [cx.sb("mt3%d" % i, [128, 512], F32) for i in range(2)]
    ob = [cx.sb("mob%d" % i, [128, 512], BF16) for i in range(2)]
    kr = ((0, 4), (4, 8), (8, 16))
    n = 0
    for dmt in range(16):
        for tb in range(4):
            i = n % 2
            g_ = gt[i]
            cx.dma(g_[:], dram_ap(T["gatesT"], (dmt * 128) * TOK + tb * 512, [[TOK, 128], [D * TOK, 3], [1, 512]]), g_, T["gatesT"])
            tt = (t1[i], t2[i], t3[i])
            for br in range(3):
                p = ps[i * 3 + br]
                k0, k1 = kr[br]
                for k in range(k0, k1):
                    cx.op("pe", lambda e: e.matmul(out=p[:], lhsT=wup[:, k, dmt * 128:(dmt + 1) * 128],
                                                   rhs=aT[:, k, tb * 512:(tb + 1) * 512], start=(k == k0), stop=(k == k1 - 1)),
                          reads=[wup, aT], writes=[p])
                cx.op("dve", lambda e: e.tensor_tensor(out=tt[br][:], in0=p[:], in1=g_[:, br, :], op=ALU.mult),
                      reads=[p, g_], writes=[tt[br]])
            cx.op("pool", lambda e: e.tensor_tensor(out=tt[0][:], in0=tt[0][:], in1=tt[1][:], op=ALU.add),
                  reads=[tt[0], tt[1]], writes=[tt[0]])
            cx.op("pool", lambda e: e.tensor_tensor(out=ob[i][:], in0=tt[0][:], in1=tt[2][:], op=ALU.add),
                  reads=[tt[0], tt[2]], writes=[ob[i]])
            cx.dma(T["mergedT"].t.ap()[dmt * 128:(dmt + 1) * 128, tb * 512:(tb + 1) * 512], ob[i][:], T["mergedT"], ob[i], q="pool")
            n += 1
    cx.end_phase()


def phase_wo(cx, T, layer, x_src, x_dst):
    cx.begin_phase()
    mT = cx.sb("omT", [128, 16, TOK], BF16)
    cx.dma(mT[:], dram_ap(T["mergedT"], 0, [[TOK, 128], [128 * TOK, 16], [1, TOK]]), mT, T["mergedT"])
    stg = [cx.sb("ostg%d" % i, [128, 16, 512], F32) for i in range(2)]
    wbf = [cx.sb("owbf%d" % i, [128, 16, 512], BF16) for i in range(2)]
    xs = [cx.sb("oxs%d" % i, [128, 512], F32) for i in range(3)]
    ps = [cx.ps("ops%d" % i, [128, 512], F32) for i in range(4)]
    w = T["w_o"]
    w.rowlen = D
    w.base = T["wl"][layer] * D * D
    n = 0
    load_w_bf16(cx, w, 0, 0, 512, stg[0], wbf[0], "pool")
    for cb in range(4):
        if cb + 1 < 4:
            load_w_bf16(cx, w, 0, (cb + 1) * 512, 512, stg[(cb + 1) % 2], wbf[(cb + 1) % 2], "pool")
        w_ = wbf[cb % 2]
        for tt in range(NT):
            p = ps[n % 4]
            x_ = xs[n % 3]
            n += 1
            cx.dma(x_[:], x_src.t.ap()[tt * 128:(tt + 1) * 128, cb * 512:(cb + 1) * 512], x_, x_src)
            for k in range(16):
                cx.op("pe", lambda e: e.matmul(out=p[:], lhsT=mT[:, k, tt * 128:(tt + 1) * 128], rhs=w_[:, k, :],
                                               start=(k == 0), stop=(k == 15)), reads=[mT, w_], writes=[p])
            cx.op("dve", lambda e: e.tensor_tensor(out=x_[:], in0=p[:], in1=x_[:], op=ALU.add), reads=[p, x_], writes=[x_])
            cx.dma(x_dst.t.ap()[tt * 128:(tt + 1) * 128, cb * 512:(cb + 1) * 512], x_[:], x_dst, x_, q="pool")
    cx.end_phase()


def phase_peer_q(cx, T, layer, x_src):
    cx.begin_phase()
    hT = cx.sb("phT", [128, 16, TOK], BF16)
    rmsnorm_to_hT(cx, x_src, (T["ffn_norm_g"], T["wl"][layer] * D), hT, T["G"]["ident_bf"], T)
    stg = [cx.sb("pstg%d" % i, [128, 16, 512], F32) for i in range(2)]
    wbf = [cx.sb("pwbf%d" % i, [128, 16, 512], BF16) for i in range(2)]
    ps = [cx.ps("pps%d" % i, [128, 512], F32) for i in range(4)]
    of = [cx.sb("pof%d" % i, [128, 512], F32) for i in range(3)]
    w = T["peer_wq"]
    w.rowlen = D
    w.base = T["wl"][layer] * D * D
    n = 0
    load_w_bf16(cx, w, 0, 0, 512, stg[0], wbf[0], "pool")
    for cb in range(4):
        if cb + 1 < 4:
            load_w_bf16(cx, w, 0, (cb + 1) * 512, 512, stg[(cb + 1) % 2], wbf[(cb + 1) % 2], "pool")
        w_ = wbf[cb % 2]
        for m in range(4):
            for tb in range(4):
                p = ps[n % 4]
                o = of[n % 3]
                n += 1
                for k in range(16):
                    cx.op("pe", lambda e: e.matmul(out=p[:], lhsT=w_[:, k, m * 128:(m + 1) * 128], rhs=hT[:, k, tb * 512:(tb + 1) * 512],
                                                   start=(k == 0), stop=(k == 15)), reads=[w_, hT], writes=[p])
                if n % 2 == 0:
                    cx.op("act", lambda e: e.copy(out=o[:], in_=p[:]), reads=[p], writes=[o])
                else:
                    cx.op("dve", lambda e: e.tensor_copy(out=o[:], in_=p[:]), reads=[p], writes=[o])
                row = cb * 512 + m * 128
                cx.dma(T["qT"].t.ap()[row:row + 128, tb * 512:(tb + 1) * 512], o[:], T["qT"], o, q="pool")
    cx.end_phase()


def phase_peer(cx, T, layer, x_src, x_dst):
    G = T["G"]
    cx.begin_phase()
    NEG = -1.0e30
    sk = cx.sb("sk", [128, 2, 128], F32)
    cx.dma(sk[:], dram_ap(T["peer_subkeys"], T["wl"][layer] * 2 * 128 * 128, [[128, 128], [128 * 128, 2], [1, 128]]), sk, T["peer_subkeys"])
    scp = [cx.ps("scp%d" % i, [128, 512], F32) for i in range(4)]
    acc = [cx.ps("acc%d" % i, [128, 512], F32) for i in range(4)]
    skT = cx.sb("skT", [128, 2, 128], F32)
    for p_ in range(2):
        cx.op("pe", lambda e: e.transpose(out=scp[0][:, p_ * 128:(p_ + 1) * 128], in_=sk[:, p_, :], identity=G["ident_f"][:]),
              reads=[sk, G["ident_f"]], writes=[scp[0]])
    cx.op("act", lambda e: e.copy(out=skT[:].rearrange("p a b -> p (a b)"), in_=scp[0][:, 0:256]), reads=[scp[0]], writes=[skT])
    ids_l = [cx.sb("ids%d" % i, [128, 128], I32) for i in range(NT)]
    gat_l = [cx.sb("gat%d" % i, [128, 128], F32) for i in range(NT)]
    qTt = [cx.sb("qTt%d" % i, [128, 16, 128], F32) for i in range(2)]
    sc = cx.sb("sc", [128, 16, 128], F32)
    scr = cx.sb("scr", [128, 256], F32)
    m8 = cx.sb("m8", [128, 16, 16], F32)
    i8 = cx.sb("i8", [128, 16, 16], U32)
    i8f = cx.sb("i8f", [128, 16, 16], F32)
    cand = cx.sb("cand", [128, 8, 256], F32)
    bs = cx.sb("bs", [128, 8, 16], F32)
    bj = cx.sb("bj", [128, 8, 16], U32)
    ja = cx.sb("ja", [128, 8, 16], U32)
    jb = cx.sb("jb", [128, 8, 16], U32)
    jaf = cx.sb("jaf", [128, 8, 16], F32)
    jbf = cx.sb("jbf", [128, 8, 16], F32)
    eq = cx.sb("eq", [128, 8, 16, 16], F32)
    sel1 = cx.sb("sel1", [128, 8, 16], F32)
    sel2 = cx.sb("sel2", [128, 8, 16], F32)
    ex = cx.sb("ex", [128, 8, 16], F32)
    esum = cx.sb("esum", [128, 8], F32)
    m8v = m8[:].rearrange("p (h two) k -> p h two k", two=2)
    i8v = i8f[:].rearrange("p (h two) k -> p h two k", two=2)
    B4 = [128, 8, 16, 16]
    def topk_gen(tt):
            q_ = qTt[tt % 2]
            cx.dma(q_[:], dram_ap(T["qT"], tt * 128, [[TOK, 128], [128 * TOK, 16], [1, 128]]), q_, T["qT"])
            yield
            for hp in range(16):
                cx.op("pe", lambda e: e.matmul(out=scp[hp // 4][:, (hp % 4) * 128:(hp % 4 + 1) * 128], lhsT=q_[:, hp, :],
                                               rhs=skT[:, hp % 2, :], start=True, stop=True, skip_group_check=True),
                      reads=[q_, skT], writes=[scp[hp // 4]])
                yield
            for b4 in range(4):
                cx.op("act", lambda e: e.copy(out=sc[:, b4 * 4:(b4 + 1) * 4, :].rearrange("p a b -> p (a b)"), in_=scp[b4][:]),
                      reads=[scp[b4]], writes=[sc])
                yield
            for hp in range(16):
                cx.op("dve", lambda e: e.max(out=m8[:, hp, 0:8], in_=sc[:, hp, :]), reads=[sc], writes=[m8])
                yield
                cx.op("dve", lambda e: e.match_replace(out=scr[:, 0:128], in_to_replace=m8[:, hp, 0:8], in_values=sc[:, hp, :],
                                                       imm_value=NEG), reads=[m8, sc], writes=[scr])
                yield
                cx.op("dve", lambda e: e.max(out=m8[:, hp, 8:16], in_=scr[:, 0:128]), reads=[scr], writes=[m8])
                yield
                cx.op("dve", lambda e: e.max_index(out=i8[:, hp, 0:8], in_max=m8[:, hp, 0:8], in_values=sc[:, hp, :]),
                      reads=[m8, sc], writes=[i8])
                yield
                cx.op("dve", lambda e: e.max_index(out=i8[:, hp, 8:16], in_max=m8[:, hp, 8:16], in_values=sc[:, hp, :]),
                      reads=[m8, sc], writes=[i8])
                yield
            cx.op("dve", lambda e: e.tensor_copy(out=i8f[:], in_=i8[:]), reads=[i8], writes=[i8f])
            yield
            cx.op("dve", lambda e: e.tensor_tensor(out=cand[:].rearrange("p h (a b) -> p h a b", b=16),
                                                   in0=m8v[:, :, 0, :].unsqueeze(3).broadcast_to(B4),
                                                   in1=m8v[:, :, 1, :].unsqueeze(2).to_broadcast(B4), op=ALU.add),
                  reads=[m8], writes=[cand])
            yield
            for h in range(8):
                cx.op("dve", lambda e: e.max(out=bs[:, h, 0:8], in_=cand[:, h, :]), reads=[cand], writes=[bs])
                yield
                cx.op("dve", lambda e: e.match_replace(out=scr[:], in_to_replace=bs[:, h, 0:8], in_values=cand[:, h, :],
                                                       imm_value=NEG), reads=[bs, cand], writes=[scr])
                yield
                cx.op("dve", lambda e: e.max(out=bs[:, h, 8:16], in_=scr[:]), reads=[scr], writes=[bs])
                yield
                cx.op("dve", lambda e: e.max_index(out=bj[:, h, 0:8], in_max=bs[:, h, 0:8], in_values=cand[:, h, :]),
                      reads=[bs, cand], writes=[bj])
                yield
                cx.op("dve", lambda e: e.max_index(out=bj[:, h, 8:16], in_max=bs[:, h, 8:16], in_values=cand[:, h, :]),
                      reads=[bs, cand], writes=[bj])
                yield
            cx.op("dve", lambda e: e.tensor_single_scalar(out=ja[:], in_=bj[:], scalar=4, op=ALU.logical_shift_right),
                  reads=[bj], writes=[ja])
            yield
            cx.op("dve", lambda e: e.tensor_single_scalar(out=jb[:], in_=bj[:], scalar=15, op=ALU.bitwise_and),
                  reads=[bj], writes=[jb])
            yield
            cx.op("dve", lambda e: e.tensor_copy(out=jaf[:], in_=ja[:]), reads=[ja], writes=[jaf])
            yield
            cx.op("dve", lambda e: e.tensor_copy(out=jbf[:], in_=jb[:]), reads=[jb], writes=[jbf])
            yield
            iob = G["iota16"][:].unsqueeze(1).unsqueeze(1).to_broadcast(B4)
            for (jf, pp, sel) in ((jaf, 0, sel1), (jbf, 1, sel2)):
                cx.op("dve", lambda e: e.tensor_tensor(out=eq[:], in0=jf[:].unsqueeze(3).broadcast_to(B4), in1=iob, op=ALU.is_equal),
                      reads=[jf, G["iota16"]], writes=[eq])
                yield
                cx.op("dve", lambda e: e.tensor_tensor(out=eq[:], in0=eq[:], in1=i8v[:, :, pp, :].unsqueeze(2).to_broadcast(B4),
                                                       op=ALU.mult), reads=[eq, i8f], writes=[eq])
                yield
                cx.op("dve", lambda e: e.tensor_reduce(out=sel[:], in_=eq[:], axis=AX.X, op=ALU.add), reads=[eq], writes=[sel])
                yield
            cx.op("dve", lambda e: e.tensor_scalar(out=sel1[:], in0=sel1[:], scalar1=128.0, scalar2=float(T["wl"][layer] * 16384),
                                                   op0=ALU.mult, op1=ALU.add), reads=[sel1], writes=[sel1])
            yield
            cx.op("dve", lambda e: e.tensor_tensor(out=sel1[:], in0=sel1[:], in1=sel2[:], op=ALU.add), reads=[sel1, sel2], writes=[sel1])
            yield
            cx.op("dve", lambda e: e.tensor_copy(out=ids_l[tt][:], in_=sel1[:].rearrange("p h k -> p (h k)")),
                  reads=[sel1], writes=[ids_l[tt]])
            yield
            cx.op("dve", lambda e: e.tensor_tensor(out=ex[:], in0=bs[:], in1=bs[:, :, 0:1].broadcast_to([128, 8, 16]), op=ALU.subtract),
                  reads=[bs], writes=[ex])
            yield
            cx.op("act", lambda e: e.activation(out=ex[:], in_=ex[:], func=AF.Exp), reads=[ex], writes=[ex])
            yield
            cx.op("dve", lambda e: e.tensor_reduce(out=esum[:], in_=ex[:], axis=AX.X, op=ALU.add), reads=[ex], writes=[esum])
            yield
            cx.op("dve", lambda e: e.reciprocal(out=esum[:], in_=esum[:]), reads=[esum], writes=[esum])
            yield
            cx.op("dve", lambda e: e.tensor_tensor(out=gat_l[tt][:].rearrange("p (h k) -> p h k", k=16), in0=ex[:],
                                                   in1=esum[:].unsqueeze(2).broadcast_to([128, 8, 16]), op=ALU.mult),
                  reads=[ex, esum], writes=[gat_l[tt]])
            yield
    NB = 6
    uvb = [cx.sb("uvb%d" % i, [128, 2 * D], BF16) for i in range(NB)]
    xt = [cx.sb("pxt%d" % i, [128, D], F32) for i in range(2)]
    hf = [cx.sb("phf%d" % i, [128, D], BF16) for i in range(2)]
    gb = cx.sb("pgb", [128, D], F32)
    cx.dma(gb[:], dram_ap(T["ffn_norm_g"], T["wl"][layer] * D, [[0, 128], [1, D]]), gb, T["ffn_norm_g"])
    junkp = cx.sb("pjunkp", [128, D], BF16)
    junk2 = [cx.sb("pjunk%d" % i, [128, D], BF16) for i in range(2)]
    st = [cx.sb("pst%d" % i, [128, 4], F32) for i in range(2)]
    pre_s = [cx.sb("ppre%d" % i, [128, 1], F32) for i in range(4)]
    gl_s = [cx.sb("pgl%d" % i, [128, 1], F32) for i in range(4)]
    dg = [cx.sb("pdg%d" % i, [128, 128], BF16) for i in range(4)]
    uvtab = T["uv16"].t.ap()
    idm = G["ident_bf"]

    def prep(tt):
        i = tt % 2
        x_t, s_t, h_f = xt[i], st[i], hf[i]
        cx.dma(x_t[:], x_src.t.ap()[tt * 128:(tt + 1) * 128, :], x_t, x_src)
        cx.op("act", lambda e: e.activation(out=junkp[:], in_=x_t[:], func=AF.Square, accum_out=s_t[:, 0:1]),
              reads=[x_t], writes=[junkp, s_t])
        cx.op("dve", lambda e: e.tensor_scalar(out=s_t[:, 1:2], in0=s_t[:, 0:1], scalar1=1.0 / D, scalar2=EPS,
                                               op0=ALU.mult, op1=ALU.add), reads=[s_t], writes=[s_t])
        cx.op("act", lambda e: e.sqrt(out=s_t[:, 2:3], in_=s_t[:, 1:2]), reads=[s_t], writes=[s_t])
        cx.op("dve", lambda e: e.reciprocal(out=s_t[:, 3:4], in_=s_t[:, 2:3]), reads=[s_t], writes=[s_t])
        cx.op("dve", lambda e: e.scalar_tensor_tensor(out=h_f[:], in0=x_t[:], scalar=s_t[:, 3:4], in1=gb[:],
                                                      op0=ALU.mult, op1=ALU.mult), reads=[x_t, s_t, gb], writes=[h_f])

    def v_step(tt, j, uv_):
        d_ = dg[j % 4]
        gl = gl_s[j % 4]
        cx.op("dve", lambda e: e.tensor_scalar(out=d_[:], in0=idm[:], scalar1=gl[:, 0:1], scalar2=gat_l[tt][:, j:j + 1],
                                               op0=ALU.mult, op1=ALU.mult), reads=[idm, gl, gat_l[tt]], writes=[d_])
        for c in range(4):
            cx.op("pe", lambda e: e.matmul(out=acc[c][:], lhsT=d_[:], rhs=uv_[:, D + c * 512:D + (c + 1) * 512],
                                           start=(j == 0), stop=(j == 127)), reads=[d_, uv_], writes=[acc[c]])

    def fin(tt):
        x_t = xt[tt % 2]
        for c in range(4):
            cx.op("dve", lambda e: e.tensor_tensor(out=x_t[:, c * 512:(c + 1) * 512], in0=acc[c][:], in1=x_t[:, c * 512:(c + 1) * 512],
                                                   op=ALU.add), reads=[acc[c], x_t], writes=[x_t])
        cx.dma(x_dst.t.ap()[tt * 128:(tt + 1) * 128, :], x_t[:], x_dst, x_t)

    ng = 0
    for _ in topk_gen(0):
        pass
    prep(0)
    for tt in range(NT):
        i = tt % 2
        prev = None
        gen = topk_gen(tt + 1) if tt + 1 < NT else iter(())
        for j in range(128):
            next(gen, None)
            next(gen, None)
            uv_ = uvb[ng % NB]
            jk = junk2[ng % 2]
            ng += 1
            cx.dma(None, None, uv_, T["uv16"], q="pool",
                   fn=lambda e: e.indirect_dma_start(out=uv_[:], out_offset=None, in_=uvtab,
                                                     in_offset=bass.IndirectOffsetOnAxis(ap=ids_l[tt][:, j:j + 1], axis=0)),
                   extra_reads=[ids_l[tt]])
            pr = pre_s[j % 4]
            cx.op("dve", lambda e: e.scalar_tensor_tensor(out=jk[:], in0=uv_[:, 0:D], scalar=1.0, in1=hf[i][:], op0=ALU.mult,
                                                          op1=ALU.mult, accum_out=pr[:, 0:1]), reads=[uv_, hf[i]], writes=[jk, pr])
            if prev is not None:
                v_step(tt, prev[0], prev[1])
            gl = gl_s[j % 4]
            cx.op("act", lambda e: e.activation(out=gl[:], in_=pr[:], func=AF.Gelu), reads=[pr], writes=[gl])
            prev = (j, uv_)
            if j == 64 and tt + 1 < NT:
                prep(tt + 1)
        v_step(tt, prev[0], prev[1])
        for _ in gen:
            pass
        fin(tt)
    cx.end_phase()


def phase_final(cx, T, x_src, out_dst):
    cx.begin_phase()
    gb = cx.sb("fgb", [128, D], F32)
    cx.dma(gb[:], dram_ap(T["final_norm_g"], 0, [[0, 128], [1, D]]), gb, T["final_norm_g"])
    xt = [cx.sb("fxt%d" % i, [128, D], F32) for i in range(2)]
    yo = [cx.sb("fyo%d" % i, [128, D], F32) for i in range(2)]
    junk = cx.sb("fjunk", [128, D], BF16)
    st = [cx.sb("fst%d" % i, [128, 4], F32) for i in range(2)]
    for tt in range(NT):
        i = tt % 2
        x_t, s_t = xt[i], st[i]
        cx.dma(x_t[:], x_src.t.ap()[tt * 128:(tt + 1) * 128, :], x_t, x_src)
        cx.op("act", lambda e: e.activation(out=junk[:], in_=x_t[:], func=AF.Square, accum_out=s_t[:, 0:1]),
              reads=[x_t], writes=[junk, s_t])
        cx.op("dve", lambda e: e.tensor_scalar(out=s_t[:, 1:2], in0=s_t[:, 0:1], scalar1=1.0 / D, scalar2=EPS,
                                               op0=ALU.mult, op1=ALU.add), reads=[s_t], writes=[s_t])
        cx.op("act", lambda e: e.sqrt(out=s_t[:, 2:3], in_=s_t[:, 1:2]), reads=[s_t], writes=[s_t])
        cx.op("dve", lambda e: e.reciprocal(out=s_t[:, 3:4], in_=s_t[:, 2:3]), reads=[s_t], writes=[s_t])
        cx.op("dve", lambda e: e.scalar_tensor_tensor(out=yo[i][:], in0=x_t[:], scalar=s_t[:, 3:4], in1=gb[:],
                                                      op0=ALU.mult, op1=ALU.mult), reads=[x_t, s_t, gb], writes=[yo[i]])
        cx.dma(out_dst.t.ap()[tt * 128:(tt + 1) * 128, :], yo[i][:], out_dst, yo[i])
    cx.end_phase()


import ml_dtypes

BF = ml_dtypes.bfloat16


def rel_bucket_np(rel):
    rel = np.asarray(rel, dtype=np.int64)
    half, max_exact = 16, 8
    n = np.abs(rel)
    nf = np.maximum(n, 1).astype(np.float32) / np.float32(max_exact)
    big = max_exact + (np.log(nf) / np.float32(math.log(2048 / max_exact)) * np.float32(half - max_exact)).astype(np.int32)
    big = np.minimum(big, half - 1)
    return np.where(rel > 0, half, 0) + np.where(n < max_exact, n, big)


_CONST_CACHE = {}


def host_consts(half):
    if half in _CONST_CACHE:
        return _CONST_CACHE[half]
    c = {}
    c["c_ident_bf"] = np.eye(128, dtype=np.float32).astype(BF)
    c["c_ident_f"] = np.eye(128, dtype=np.float32)
    c["c_J"] = np.ascontiguousarray(np.eye(128, dtype=np.float32)[::-1])
    c["c_iota16"] = np.tile(np.arange(16, dtype=np.float32)[None, :], (128, 1))
    cc = np.arange(128)
    th = 2.0 * np.pi * ((cc[:, None] * cc[None, :]) % 128) / 128.0
    c["c_dftc"] = np.concatenate([np.cos(th), -np.sin(th)], axis=1).astype(np.float32).astype(BF)
    s_ = np.arange(SEQ, dtype=np.int64)
    k_ = half * TOK + np.arange(TOK, dtype=np.int64)
    ph = ((s_[:, None] * k_[None, :]) % SEQ).astype(np.float64) * (2.0 * np.pi / SEQ)
    c["c_dfts"] = np.stack([np.cos(ph), np.sin(ph)], axis=0).astype(np.float32).astype(BF)
    idx = np.arange(4096)
    oh = np.zeros((32, 2, 4096), np.float32)
    for s2, sh in ((0, (0 - half) * 2048), (1, (1 - half) * 2048)):
        rel = 2047 - idx + sh
        b = rel_bucket_np(rel)
        valid = idx < 4095
        oh[b[valid], s2, idx[valid]] = 1.0
    c["c_oh_diff"] = oh
    ohl = np.zeros((33, 3, 512), np.float32)
    for g, (d, rad) in enumerate(DIL):
        for t in range(2):
            for ix in range(255):
                nn = 127 - ix
                if t == 0:
                    ok = nn >= 0
                    rel = (nn - 64) * d
                else:
                    ok = nn <= 0
                    rel = (nn + 64) * d
                if ok:
                    ohl[int(rel_bucket_np(rel)), g, t * 256 + ix] = 1.0
                else:
                    ohl[32, g, t * 256 + ix] = 1.0
    c["c_oh_dil"] = ohl
    hm = np.zeros((128, 2), np.float32)
    hm[:, half] = 1.0
    c["c_half"] = hm
    _CONST_CACHE[half] = c
    return c


CONST_SPECS = [("c_ident_bf", [128, 128], BF16), ("c_ident_f", [128, 128], F32), ("c_J", [128, 128], F32),
               ("c_iota16", [128, 16], F32), ("c_dftc", [128, 256], BF16), ("c_dfts", [2, SEQ, TOK], BF16),
               ("c_oh_diff", [32, 2, 4096], F32), ("c_oh_dil", [33, 3, 512], F32), ("c_half", [128, 2], F32)]

WEIGHT_SPECS = [("mix_norm_g", [D]), ("w_in", [D, INW]), ("b_gate", [6144]), ("w_up_a", [512, D]), ("w_up_b", [512, D]),
                ("w_up_c", [1024, D]), ("diff_lambda", [4, 128]), ("diff_subln_g", [256]), ("w_o", [D, D]),
                ("ffn_norm_g", [D]), ("peer_wq", [D, D]), ("peer_subkeys", [2, 128, 128]), ("peer_u", [16384, D]),
                ("peer_v", [16384, D])]

A_OUT = [("kfT", [3072, TOK], BF16), ("qdT", [1536, TOK], BF16), ("qcT", [1024, TOK], BF16), ("vtok", [TOK, 2560], BF16),
         ("gatesT", [6144, TOK], F32)]
EXCH = [("fz", [512, SEQ], BF16), ("kd_ext", [1536, 3 * TOK], BF16), ("vd_ext", [3 * TOK, 12, 129], BF16),
        ("kc_oo", [1024, SEQ], BF16), ("vc_oo", [SEQ, 1024], BF16)]


def phase_exchange(cx, T):
    G = T["G"]
    if not T.get("ag_done"):
        for c in range(6):
            cx.allgather(T["kfT"], T["kfT"].t.ap()[c * 512:(c + 1) * 512, :], T["kf_g"], T["kf_g"].t.ap()[c * 1024:(c + 1) * 1024, :])
        for c in range(8):
            cx.allgather(T["vtok"], T["vtok"].t.ap()[c * 256:(c + 1) * 256, :], T["v_g"], T["v_g"].t.ap()[c * 512:(c + 1) * 512, :])
    T["ag_done"] = False
    cx.begin_phase()
    src = [cx.sb("xv%d" % i, [128, 1536], BF16) for i in range(3)]
    dst = [cx.sb("xo%d" % i, [128, 12, 129], BF16) for i in range(3)]
    ones = cx.sb("xones", [128, 12], F32)
    cx.op("dve", lambda e: e.memset(ones[:], 1.0), writes=[ones])
    n = 0
    for region in range(3):
        for tile in range(NT):
            s_, d_ = src[n % 3], dst[n % 3]
            n += 1
            if region == 1:
                cx.dma(s_[:], T["vtok"].t.ap()[tile * 128:(tile + 1) * 128, 0:1536], s_, T["vtok"])
                cx.op("dve", lambda e: e.tensor_copy(out=d_[:, :, 0:128], in_=s_[:].rearrange("p (h e) -> p h e", e=128)),
                      reads=[s_], writes=[d_])
                cx.op("dve", lambda e: e.tensor_copy(out=d_[:, :, 128], in_=ones[:]), reads=[ones, d_], writes=[d_])
            else:
                r = 0 if region == 0 else 1
                msk = G["half"][:, 1:2] if region == 0 else G["half"][:, 0:1]
                row = (tile // 2) * 512 + r * 256 + (tile % 2) * 128
                cx.dma(s_[:], T["v_g"].t.ap()[row:row + 128, 0:1536], s_, T["v_g"])
                cx.op("dve", lambda e: e.tensor_scalar(out=d_[:, :, 0:128], in0=s_[:].rearrange("p (h e) -> p h e", e=128),
                                                       scalar1=msk, scalar2=None, op0=ALU.mult),
                      reads=[s_, G["half"]], writes=[d_])
                cx.op("dve", lambda e: e.tensor_scalar(out=d_[:, :, 128], in0=ones[:], scalar1=msk, scalar2=None, op0=ALU.mult),
                      reads=[ones, G["half"], d_], writes=[d_])
            r0 = region * TOK + tile * 128
            cx.dma(T["vd_ext"].t.ap()[r0:r0 + 128, :, :], d_[:], T["vd_ext"], d_)
    cx.end_phase()


def build_fused():
    nc = bass.Bass("TRN2", target_bir_lowering=False)
    cx = Ctx(nc)
    T = {}

    def ext_in(name, shape, dt):
        T[name] = cx.dram(name, shape, dt, "ExternalInput")

    def scratch(name, shape, dt):
        T[name] = cx.dram(name, shape, dt, "ExternalOutput" if (DEBUG_OUT and name in DEBUG_OUT) else "Internal")

    for name, shape, dt in CONST_SPECS:
        ext_in(name, shape, dt)
    ext_in("x_in", [TOK, D], F32)
    ext_in("rel_bias", [32, 16], F32)
    ext_in("final_norm_g", [D], F32)
    for name, shape in WEIGHT_SPECS:
        ext_in(name, [2] + shape, F32)
    T["out"] = cx.dram("out", [TOK, D], F32, "ExternalOutput")
    for name, shape, dt in A_OUT:
        scratch(name, shape, dt)
    for l in range(2):
        scratch("kf_g%d" % l, [6 * 2 * 512, TOK], BF16)
        scratch("v_g%d" % l, [8 * 2 * 256, 2560], BF16)
    scratch("vd_ext", [3 * TOK, 12, 129], BF16)
    scratch("rb_diff", [4, 2, 4096], F32)
    scratch("rows_dil", [3, 12, 512], F32)
    scratch("strip_diff", [4, 2, 128, DIFF_W], F32)
    scratch("tm_dil", [12, 128, 256], F32)
    scratch("numd", [3, TOK, 4, 129], F32)
    scratch("mixT", [2048, TOK], BF16)
    scratch("mergedT", [D, TOK], BF16)
    scratch("qT", [D, TOK], F32)
    scratch("xb", [TOK, D], F32)
    scratch("xc", [TOK, D], F32)
    if PEER_BF16:
        scratch("uv16", [2 * 16384, 2 * D], BF16)
    T["wl"] = {0: 0, 1: 1}
    T["G"] = load_consts(cx, T)
    phase_bias(cx, T)
    x_cur = T["x_in"]
    for layer in range(NLAYERS):
        T["kf_g"] = T["kf_g%d" % layer]
        T["v_g"] = T["v_g%d" % layer]
        phase_A(cx, layer, x_cur, T)
        phase_exchange(cx, T)
        phase_fnet(cx, T)
        phase_dil(cx, T)
        phase_diff(cx, T, layer)
        phase_merge(cx, T, layer)
        phase_wo(cx, T, layer, x_cur, T["xb"])
        phase_peer_q(cx, T, layer, T["xb"])
        phase_peer(cx, T, layer, T["xb"], T["xc"])
        x_cur = T["xc"]
    phase_final(cx, T, x_cur, T["out"])
    cx.finish()
    return nc


NLAYERS = 2
PEER_BF16 = True
_PROG = {}


def kernel(**inputs):
    inp = {k: np.asarray(v) for k, v in inputs.items()}
    x = inp["x"].astype(np.float32, copy=False)
    cores = list(range(8))
    if "p" not in _PROG:
        _PROG["p"] = build_fused()
    maps = []
    for c in cores:
        m = dict(host_consts(c % 2))
        m["rel_bias"] = inp["rel_bias"]
        m["final_norm_g"] = inp["final_norm_g"]
        for name, shape in WEIGHT_SPECS:
            m[name] = inp[name]
        m["x_in"] = np.ascontiguousarray(x[c // 2, (c % 2) * TOK:(c % 2 + 1) * TOK])
        maps.append(m)
    res = run_bass_kernel_spmd(_PROG["p"], maps, core_ids=cores).results
    out = np.empty((4, SEQ, D), np.float32)
    for c in cores:
        out[c // 2, (c % 2) * TOK:(c % 2 + 1) * TOK] = res[c]["out"]
    return out
```
